# Optimizing a Trainium2 kernel written in Bass

```python
import jax, jax.numpy as jnp
from jax import lax
import numpy as np

D_MODEL = 1024
BATCH = 8
SEQ = 2048
DEPTH = 1
DEC_BATCH = 128
DEC_SEQ = 4
PAST_LEN = 16384
PAGE_SIZE = 128

D_A = D_MODEL // 2
D_B = D_MODEL // 2
CONV_A_WIDTH = 3
CONV_B_WIDTH = 31
N_GROUPS = 4
EXPERTS_PER_GROUP = 4
N_EXPERTS = N_GROUPS * EXPERTS_PER_GROUP
TOP_K = 2
D_EXPERT = 256
N_MOD = 6
EPS = 1e-6
SPLIT_SIZES = (D_A, D_A, D_A, D_B, D_B, D_MODEL, D_MODEL)
SPLIT_IDX = tuple(int(s) for s in np.cumsum(SPLIT_SIZES)[:-1])
D_IN = int(sum(SPLIT_SIZES))

kernel_name = "hybrid_shortconv_conformer_hmoe_step"


def _rmsnorm(x, g):
    xf = x.astype(jnp.float32)
    xf = xf * lax.rsqrt(jnp.mean(xf * xf, axis=-1, keepdims=True) + EPS)
    return (xf * g.astype(jnp.float32)).astype(x.dtype)


def _layernorm(x, g, b):
    xf = x.astype(jnp.float32)
    mu = jnp.mean(xf, axis=-1, keepdims=True)
    var = jnp.mean(jnp.square(xf - mu), axis=-1, keepdims=True)
    y = (xf - mu) * lax.rsqrt(var + EPS) * g.astype(jnp.float32) + b.astype(jnp.float32)
    return y.astype(x.dtype)


def _causal_dwconv(u, buf, w):
    k = w.shape[0]
    full = jnp.concatenate([buf.astype(u.dtype), u], axis=1)
    out = lax.conv_general_dilated(
        full, w[:, None, :].astype(u.dtype), window_strides=(1,), padding='VALID',
        dimension_numbers=('NWC', 'WIO', 'NWC'), feature_group_count=u.shape[-1])
    return out, full[:, full.shape[1] - (k - 1):]


def _hier_moe(h, w_group, b_group, w_expert, b_expert, w1, w3, w2):
    shp = h.shape
    t = h.reshape(-1, shp[-1])
    gl = (t @ w_group + b_group).astype(jnp.float32)
    gp = jax.nn.softmax(gl, axis=-1)
    g_idx = jnp.argmax(gl, axis=-1)
    p_g = jnp.take_along_axis(gp, g_idx[:, None], axis=-1)
    el = (t @ w_expert + b_expert).astype(jnp.float32).reshape(-1, N_GROUPS, EXPERTS_PER_GROUP)
    el = jnp.take_along_axis(el, g_idx[:, None, None], axis=1)[:, 0]
    top_v, top_i = lax.top_k(el, TOP_K)
    wts = jax.nn.softmax(top_v, axis=-1) * p_g
    ids = g_idx[:, None] * EXPERTS_PER_GROUP + top_i
    combine = jnp.sum(jax.nn.one_hot(ids, N_EXPERTS, dtype=jnp.float32) * wts[..., None], axis=1)
    a = jax.nn.silu(jnp.einsum('nd,edf->nef', t, w1)) * jnp.einsum('nd,edf->nef', t, w3)
    a = a * combine.astype(t.dtype)[..., None]
    y = jnp.einsum('nef,efd->nd', a, w2)
    return y.reshape(shp)


def _layer(x, c, buf_a, buf_b, w_ada, b_ada, norm1_g, norm2_g, w_in, conv_a_w, w_out_a,
           conv_b_w, conv_b_bias, ln_b_g, ln_b_b, w_out_b, w_o,
           w_group, b_group, w_expert, b_expert, w1, w3, w2):
    mod = jax.nn.silu(c) @ w_ada + b_ada
    sh1, sc1, gt1, sh2, sc2, gt2 = jnp.split(mod[:, None, :], N_MOD, axis=-1)
    h = _rmsnorm(x, norm1_g) * (1 + sc1) + sh1
    proj = h @ w_in
    b_a, c_a, h_a, v_b, g_b, mg_a, mg_b = jnp.split(proj, SPLIT_IDX, axis=-1)
    conv_a, new_a = _causal_dwconv(c_a * h_a, buf_a, conv_a_w)
    y_a = (b_a * conv_a) @ w_out_a
    conv_b, new_b = _causal_dwconv(v_b * jax.nn.sigmoid(g_b), buf_b, conv_b_w)
    y_b = jax.nn.silu(_layernorm(conv_b + conv_b_bias, ln_b_g, ln_b_b)) @ w_out_b
    merged = jax.nn.sigmoid(mg_a) * y_a + jax.nn.sigmoid(mg_b) * y_b
    x = x + gt1 * (merged @ w_o)
    h2 = _rmsnorm(x, norm2_g) * (1 + sc2) + sh2
    x = x + gt2 * _hier_moe(h2, w_group, b_group, w_expert, b_expert, w1, w3, w2)
    return x, new_a, new_b


def setup_inputs(seed: int = 0) -> dict:
    key = jax.random.key(seed)
    ks = jax.random.split(key, 32)
    f = jnp.float32
    nrm = lambda k, shape, s: jax.random.normal(k, shape, f) * s
    L = DEPTH
    return {
        "x_prompt": nrm(ks[0], (BATCH, SEQ, D_MODEL), 1.0),
        "x_sample": nrm(ks[1], (DEC_BATCH, DEC_SEQ, D_MODEL), 1.0),
        "c_prompt": nrm(ks[2], (BATCH, D_MODEL), 1.0),
        "c_sample": nrm(ks[3], (DEC_BATCH, D_MODEL), 1.0),
        "state_conv_a": nrm(ks[4], (L, DEC_BATCH, CONV_A_WIDTH - 1, D_A), 1.0),
        "state_conv_b": nrm(ks[5], (L, DEC_BATCH, CONV_B_WIDTH - 1, D_B), 1.0),
        "w_ada": nrm(ks[6], (L, D_MODEL, N_MOD * D_MODEL), 0.3 * D_MODEL ** -0.5),
        "b_ada": nrm(ks[7], (L, N_MOD * D_MODEL), 0.02),
        "norm1_g": 1.0 + nrm(ks[8], (L, D_MODEL), 0.02),
        "norm2_g": 1.0 + nrm(ks[9], (L, D_MODEL), 0.02),
        "w_in": nrm(ks[10], (L, D_MODEL, D_IN), D_MODEL ** -0.5),
        "conv_a_w": nrm(ks[11], (L, CONV_A_WIDTH, D_A), CONV_A_WIDTH ** -0.5),
        "w_out_a": nrm(ks[12], (L, D_A, D_MODEL), D_A ** -0.5),
        "conv_b_w": nrm(ks[13], (L, CONV_B_WIDTH, D_B), CONV_B_WIDTH ** -0.5),
        "conv_b_bias": nrm(ks[14], (L, D_B), 0.02),
        "ln_b_g": 1.0 + nrm(ks[15], (L, D_B), 0.02),
        "ln_b_b": nrm(ks[16], (L, D_B), 0.02),
        "w_out_b": nrm(ks[17], (L, D_B, D_MODEL), D_B ** -0.5),
        "w_o": nrm(ks[18], (L, D_MODEL, D_MODEL), D_MODEL ** -0.5),
        "w_group": nrm(ks[19], (L, D_MODEL, N_GROUPS), D_MODEL ** -0.5),
        "b_group": nrm(ks[20], (L, N_GROUPS), 0.01),
        "w_expert": nrm(ks[21], (L, D_MODEL, N_EXPERTS), D_MODEL ** -0.5),
        "b_expert": nrm(ks[22], (L, N_EXPERTS), 0.01),
        "w1": nrm(ks[23], (L, N_EXPERTS, D_MODEL, D_EXPERT), D_MODEL ** -0.5),
        "w3": nrm(ks[24], (L, N_EXPERTS, D_MODEL, D_EXPERT), D_MODEL ** -0.5),
        "w2": nrm(ks[25], (L, N_EXPERTS, D_EXPERT, D_MODEL), D_EXPERT ** -0.5),
        "final_norm_g": 1.0 + nrm(ks[26], (D_MODEL,), 0.02),
    }


def reference(x_prompt, x_sample, c_prompt, c_sample, state_conv_a, state_conv_b,
              w_ada, b_ada, norm1_g, norm2_g, w_in, conv_a_w, w_out_a,
              conv_b_w, conv_b_bias, ln_b_g, ln_b_b, w_out_b, w_o,
              w_group, b_group, w_expert, b_expert, w1, w3, w2, final_norm_g):
    n_p = x_prompt.shape[0]
    xp, xs = x_prompt, x_sample
    pa, pb, sa, sb = [], [], [], []
    for l in range(DEPTH):
        params = (w_ada[l], b_ada[l], norm1_g[l], norm2_g[l], w_in[l], conv_a_w[l], w_out_a[l],
                  conv_b_w[l], conv_b_bias[l], ln_b_g[l], ln_b_b[l], w_out_b[l], w_o[l],
                  w_group[l], b_group[l], w_expert[l], b_expert[l], w1[l], w3[l], w2[l])
        buf_a0 = jnp.zeros((n_p, CONV_A_WIDTH - 1, D_A), xp.dtype)
        buf_b0 = jnp.zeros((n_p, CONV_B_WIDTH - 1, D_B), xp.dtype)
        xp, na_p, nb_p = _layer(xp, c_prompt, buf_a0, buf_b0, *params)
        xs, na_s, nb_s = _layer(xs, c_sample, state_conv_a[l], state_conv_b[l], *params)
        pa.append(na_p); pb.append(nb_p); sa.append(na_s); sb.append(nb_s)
    y_prompt = _rmsnorm(xp, final_norm_g)
    y_sample = _rmsnorm(xs, final_norm_g)
    return (y_prompt, y_sample, jnp.stack(pa), jnp.stack(pb), jnp.stack(sa), jnp.stack(sb))
```

```python
from contextlib import ExitStack

import numpy as np

import concourse.bass as bass
import concourse.mybir as mybir
from concourse.bass_utils import run_bass_kernel_spmd

F32 = mybir.dt.float32
BF16 = mybir.dt.bfloat16
AF = mybir.ActivationFunctionType
ALU = mybir.AluOpType
AX = mybir.AxisListType

PE, ACT, DVE, POOL, SP = "pe", "act", "dve", "pool", "sp"
ENGINES = (PE, ACT, DVE, POOL, SP)

NCORES = 8
D = 1024
NDC = 8
SEQ = 2048
NSEQ_S = 16
TS = 4
NT = SEQ + NSEQ_S * TS
NQ = 1 + NSEQ_S
D_IN = 4608
NE = 16
DE = 256
EPS = 1e-6
BIG = 1.0e30

V_BADA = 0
V_N1G = 48
V_N2G = 56
V_FNG = 64
V_CAW = 72
V_CBW = 84
V_CBB = 208
V_LNG = 212
V_LNB = 216
V_BR = 220
NV = 240


class Op:
    __slots__ = ("eng", "fn", "deps", "pos", "is_dma", "ndma", "semkey", "needs_inc", "seq", "target", "name")


class Sched:
    def __init__(self):
        self.prog = {e: [] for e in ENGINES}
        self.last_w = {}
        self.readers = {}
        self.dma_count = {}

    def add(self, eng, fn, reads=(), writes=(), dma=None, ndma=1, name=""):
        op = Op()
        op.eng, op.fn, op.name = eng, fn, name
        op.is_dma = dma is not None
        op.semkey, op.ndma = dma, ndma
        deps = set()
        for k in reads:
            w = self.last_w.get(k)
            if w is not None:
                deps.add(w)
        for k in writes:
            w = self.last_w.get(k)
            if w is not None:
                deps.add(w)
            for r in self.readers.get(k, ()):
                deps.add(r)
        for k in reads:
            self.readers.setdefault(k, []).append(op)
        for k in writes:
            self.last_w[k] = op
            self.readers[k] = []
        deps.discard(op)
        op.deps = deps
        op.pos = len(self.prog[eng])
        op.needs_inc = False
        op.seq = None
        op.target = None
        if op.is_dma:
            c = self.dma_count.get(dma, 0) + 16 * ndma
            self.dma_count[dma] = c
            op.target = c
        self.prog[eng].append(op)
        return op

    @staticmethod
    def needs_wait(op, d):
        if d.is_dma:
            return True
        if d.eng == op.eng:
            if d.eng == PE:
                return False
            return True
        return True

    def finalize(self):
        for eng in ENGINES:
            for op in self.prog[eng]:
                for d in op.deps:
                    if (not d.is_dma) and self.needs_wait(op, d):
                        d.needs_inc = True
        for eng in ENGINES:
            c = 0
            for op in self.prog[eng]:
                if op.needs_inc and not op.is_dma:
                    c += 1
                    op.seq = c

    def emit_engine(self, eng, engobj, eng_sems, dma_sems):
        waited = {}
        for op in self.prog[eng]:
            need = {}
            for d in op.deps:
                if d.is_dma:
                    key, val = ("dma", d.semkey), d.target
                elif self.needs_wait(op, d):
                    key, val = ("eng", d.eng), d.seq
                else:
                    continue
                if need.get(key, 0) < val:
                    need[key] = val
            for key, val in need.items():
                if waited.get(key, 0) < val:
                    sem = dma_sems[key[1]] if key[0] == "dma" else eng_sems[key[1]]
                    engobj.wait_ge(sem, val)
                    waited[key] = val
            insts = op.fn(engobj)
            if insts is None:
                insts = []
            elif not isinstance(insts, (list, tuple)):
                insts = [insts]
            if op.is_dma:
                assert len(insts) == op.ndma, (op.name, len(insts), op.ndma)
                for i in insts:
                    i.then_inc(dma_sems[op.semkey], 16)
            elif op.needs_inc:
                if len(insts) == 0:
                    insts = [engobj.nop(nofuse=True)]
                insts[-1].then_inc(eng_sems[eng], 1)


class ST:
    def __init__(self, idx, t0, n, npr):
        self.idx, self.t0, self.n, self.npr = idx, t0, n, npr


class Seg:
    def __init__(self, kind, c0, n, t0):
        self.kind, self.c0, self.n, self.t0 = kind, c0, n, t0
        self.sample = (kind == "s")


class Pass:
    def __init__(self, idx, segs, last_prompt):
        self.idx, self.segs, self.last_prompt = idx, segs, last_prompt
        self.n = sum(sg_.n for sg_ in segs)


PASSES = [Pass(0, [Seg("p", 0, 448, 0)], False), Pass(1, [Seg("p", 0, 448, 448)], False),
          Pass(2, [Seg("p", 0, 448, 896)], False), Pass(3, [Seg("p", 0, 448, 1344)], False),
          Pass(4, [Seg("p", 0, 256, 1792), Seg("s", 256, 64, 2048)], True)]

SUPERTILES = [ST(0, 0, 256, 256), ST(1, 256, 512, 512), ST(2, 768, 512, 512),
              ST(3, 1280, 512, 512), ST(4, 1792, 320, 256)]


def build_program():
    nc = bass.Bass("TRN2", target_bir_lowering=False)
    S = Sched()

    def din(name, shape, dt=F32):
        return nc.dram_tensor(name, list(shape), dt, kind="ExternalInput").ap()

    def dout(name, shape, dt=F32):
        return nc.dram_tensor(name, list(shape), dt, kind="ExternalOutput").ap()

    xT_d = din("xT", [128, NDC * NT])
    cT_d = din("cT", [128, NDC * NQ])
    sa_d = din("sa", [128, 4 * NSEQ_S * 2])
    sb_d = din("sb", [128, 4 * NSEQ_S * 30])
    vecs_d = din("vecs", [128, NV])
    ident_d = din("ident", [128, 128])
    w_ada_d = din("w_ada", [D, 6 * D])
    w_in_d = din("w_in", [D, D_IN])
    w_oa_d = din("w_out_a", [512, D])
    w_ob_d = din("w_out_b", [512, D])
    w_o_d = din("w_o", [D, D])
    wr_d = din("wr", [128, NDC * 20])
    w1_d = din("w1", [NE, D, DE])
    w3_d = din("w3", [NE, D, DE])
    w2_d = din("w2", [NE, DE, D])

    yT_d = dout("yT", [128, NDC * NT])
    nap_d = dout("nap", [128, 4 * 2])
    nbp_d = dout("nbp", [128, 4 * 30])
    nas_d = dout("nas", [128, 4 * NSEQ_S * 2])
    nbs_d = dout("nbs", [128, 4 * NSEQ_S * 30])

    NWC = 26
    wc_d = nc.dram_tensor("wcache", [NWC, 128, 2048], BF16).ap()

    w_ada_v = w_ada_d.rearrange("(kc p) n -> p kc n", p=128)
    w_in_v = w_in_d.rearrange("(kc p) n -> p kc n", p=128)
    w_oa_v = w_oa_d.rearrange("(kc p) n -> p kc n", p=128)
    w_ob_v = w_ob_d.rearrange("(kc p) n -> p kc n", p=128)
    w_o_v = w_o_d.rearrange("(kc p) n -> p kc n", p=128)
    xT_dv = xT_d.rearrange("p (c t) -> p c t", c=NDC)
    yT_dv = yT_d.rearrange("p (c t) -> p c t", c=NDC)

    es = ExitStack()
    with es:
        def sb(name, shape, dt):
            return es.enter_context(nc.sbuf_tensor(name, list(shape), dt))

        xT = sb("xT_sb", [128, NDC, NT], F32)
        vecs = sb("vecs_sb", [128, NV], F32)
        identF = sb("identF", [128, 128], F32)
        identB = sb("identB", [128, 128], BF16)
        c1024 = sb("c1024", [128, 128], BF16)
        c512 = sb("c512", [128, 128], BF16)
        modT = sb("modT", [128, 48, NQ], F32)
        gsc1 = sb("gsc1", [128, NDC, NQ], F32)
        gsc2 = sb("gsc2", [128, NDC, NQ], F32)
        cT = sb("cT_sb", [128, NDC, NQ], F32)
        scT = sb("scT", [128, NDC, NQ], BF16)
        ps_all = es.enter_context(nc.psum_tensor("ps_all", [128, 8, 512], F32))

        def PS(b, n=512):
            return ps_all[:, b, 0:n]

        def vcol(off, n=1):
            return vecs[:, off:off + n]

        def mod_scalar(which, dc, q=0):
            return modT[:, which * 8 + dc, q:q + 1]

        def bc_seq(ap3):
            return ap3.unsqueeze(2).to_broadcast([128, NSEQ_S, TS])

        def v3(ap2):
            return ap2.rearrange("p (s t) -> p s t", t=TS)


        def dma(eng, key, out_ap, in_ap, reads=(), writes=(), name=""):
            return S.add(eng, lambda e, o=out_ap, i=in_ap: [e.dma_start(out=o, in_=i)],
                         reads=reads, writes=writes, dma=key, ndma=1, name=name)

        dma(SP, "ld_vecs", vecs[:], vecs_d[:, :], writes=["vecs"], name="ld_vecs")
        dma(SP, "ld_ident", identF[:], ident_d[:, :], writes=["identF"], name="ld_ident")
        dma(SP, "ld_cT", cT[:].rearrange("p a b -> p (a b)"), cT_d[:, :], writes=["cT"], name="ld_cT")

        def xkeys(P, si):
            return [("x", P.idx, si, dc) for dc in range(NDC)]

        def load_x(P, extra_reads=()):
            for si, sg_ in enumerate(P.segs):
                dma(SP, ("ld_x", P.idx, si), xT[:, :, sg_.t0:sg_.t0 + sg_.n], xT_dv[:, :, sg_.t0:sg_.t0 + sg_.n],
                    reads=list(extra_reads), writes=xkeys(P, si), name="ld_x%d_%d" % (P.idx, si))

        load_x(PASSES[0])

        S.add(DVE, lambda e: [e.memset(c1024[:], 1.0 / 1024.0), e.memset(c512[:], 1.0 / 512.0)],
              writes=["c1024", "c512"], name="memset_consts")
        S.add(DVE, lambda e: [e.tensor_copy(identB[:], identF[:])], reads=["identF"], writes=["identB"], name="identB")
        S.add(ACT, lambda e: [e.activation(out=scT[:], in_=cT[:], func=AF.Silu)], reads=["cT"], writes=["scT"], name="silu_c")

        esA = ExitStack()
        esA.__enter__()

        def sbA(name, shape, dt):
            return esA.enter_context(nc.sbuf_tensor(name, list(shape), dt))

        NRING = 6
        ring = [sbA("ring%d" % i, [128, 2048], BF16) for i in range(NRING)]
        ring_state = {"n": 0}
        hT = sbA("hT", [128, NDC, 512], BF16)
        zsq = sbA("zsq", [128, 8, 512], BF16)
        tmp = sbA("tmp", [128, 2, 512], F32)
        rstd = sbA("rstd", [128, 512], F32)
        ba = sbA("ba", [128, 4, 512], BF16)
        ca = sbA("ca", [128, 2, 512], F32)
        sg = sbA("sg", [128, 2, 512], F32)
        ua = sbA("ua", [128, 4, 2 + 512], BF16)
        ub = sbA("ub", [128, 4, 30 + 512], BF16)
        ua_s = sbA("ua_s", [128, 4, NSEQ_S, 2 + TS], BF16)
        ub_s = sbA("ub_s", [128, 4, NSEQ_S, 30 + TS], BF16)
        vbuf = sbA("vbuf", [128, 4 * 512], F32)
        vbf = sbA("vbf", [128, 2, 512], BF16)
        vsq = sbA("vsq", [128, 2, 512], BF16)
        lnA = sbA("lnA", [128, 512], F32)
        lnB = sbA("lnB", [128, 512], F32)
        lnM = sbA("lnM", [128, 512], F32)
        sga = sbA("sga", [128, NDC, 512], BF16)
        sgb = sbA("sgb", [128, NDC, 512], BF16)
        merged = sbA("merged", [128, NDC, 512], BF16)
        mtmp = sbA("mtmp", [128, 2, 512], F32)
        dga = sbA("dga", [128, 12, 128], BF16)
        NDG = 8
        dgb = sbA("dgb", [128, NDG, 128], BF16)
        sa_t = sbA("sa_t", [128, 4, NSEQ_S, 2], F32)
        nap_t = sbA("nap_t", [128, 4, 2], F32)
        nbp_t = sbA("nbp_t", [128, 4, 30], F32)
        nas_t = sbA("nas_t", [128, 4, NSEQ_S, 2], F32)
        nbs_t = sbA("nbs_t", [128, 4, NSEQ_S, 30], F32)

        def V(c, n=512):
            return vbuf[:, c * 512:c * 512 + n]

        sb_stage = vbuf[:, 0:4 * NSEQ_S * 30].rearrange("p (c s j) -> p c s j", c=4, s=NSEQ_S)
        dma(SP, "ld_sa", sa_t[:].rearrange("p a b c -> p (a b c)"), sa_d[:, :], writes=["sa_t"], name="ld_sa")
        dma(SP, "ld_sb", vbuf[:, 0:4 * NSEQ_S * 30], sb_d[:, :], writes=[("v", c) for c in range(4)], name="ld_sb")
        S.add(DVE, lambda e: [e.tensor_copy(ua_s[:, :, :, 0:2], sa_t[:])], reads=["sa_t"], writes=["ua_s"], name="ua_s_hist")
        S.add(DVE, lambda e: [e.tensor_copy(ub_s[:, :, :, 0:30], sb_stage)],
              reads=[("v", c) for c in range(4)], writes=["ub_s"], name="ub_s_hist")
        S.add(ACT, lambda e: [e.activation(out=nbs_t[:, :, :, 0:26], in_=sb_stage[:, :, :, 4:30], func=AF.Copy)],
              reads=[("v", c) for c in range(4)], writes=["nbs_t"], name="nbs_hist")
        S.add(DVE, lambda e: [e.memset(ua[:, :, 0:2], 0.0), e.memset(ub[:, :, 0:30], 0.0)],
              writes=["ua_halo", "ub_halo"], name="halo0")
        def mk_dga(e):
            out = []
            for c in range(4):
                for k in range(3):
                    out.append(e.tensor_scalar(dga[:, c * 3 + k, :], identB[:], vcol(V_CAW + c * 3 + k), None, ALU.mult))
            return out
        S.add(DVE, mk_dga, reads=["identB", "vecs"], writes=["dga"], name="mk_dga")

        slot_pending = {}

        def ring_load(src_ap, shape3, name, store_as=None):
            slot = ring_state["n"] % NRING
            ring_state["n"] += 1
            a_, b_ = shape3
            assert a_ * b_ == 2048
            for ps_, (seq_, bid) in list(slot_pending.items()):
                if ring_state["n"] - seq_ >= 3 or ps_ == slot:
                    slot_pending.pop(ps_)
                    S.add(POOL, lambda e, bid=bid, ps_=ps_: [e.dma_start(out=wc_d[bid], in_=ring[ps_][:, :])],
                          reads=[("ring", ps_)], writes=[("wc", bid)], dma=("wcst", ps_), ndma=1, name="wc_store")
            view = ring[slot][:, 0:a_ * b_].rearrange("p (a b) -> p a b", a=a_)
            if src_ap is None:
                S.add(POOL, lambda e, slot=slot, bid=store_as: [e.dma_start(out=ring[slot][:, :], in_=wc_d[bid])],
                      reads=[("wc", store_as)], writes=[("ring", slot)], dma=("ring", slot), ndma=1, name=name)
            else:
                S.add(POOL, lambda e, o=view, i=src_ap: [e.dma_start(out=o, in_=i)],
                      writes=[("ring", slot)], dma=("ring", slot), ndma=1, name=name)
                if store_as is not None:
                    slot_pending[slot] = (ring_state["n"], store_as)
            return slot, view

        wcache = {}
        WC_BASE = {"w_in": 0, "w_o": 18, "w_oa": 22, "w_ob": 24}

        def wblk(kind, tag, idx):
            key = (kind, tag, idx)
            if key not in wcache:
                if kind == "w_ada":
                    wcache[key] = ring_load(w_ada_v[:, :, idx * 256:(idx + 1) * 256], (NDC, 256), "w_ada")
                else:
                    bid = WC_BASE[kind] + idx
                    shape = (4, 512) if kind in ("w_oa", "w_ob") else (NDC, 256)
                    if tag == 0:
                        if kind == "w_in":
                            src = w_in_v[:, :, idx * 256:(idx + 1) * 256]
                        elif kind == "w_o":
                            src = w_o_v[:, :, idx * 256:(idx + 1) * 256]
                        elif kind == "w_oa":
                            src = w_oa_v[:, :, idx * 512:(idx + 1) * 512]
                        else:
                            src = w_ob_v[:, :, idx * 512:(idx + 1) * 512]
                        wcache[key] = ring_load(src, shape, kind, store_as=bid)
                    else:
                        wcache[key] = ring_load(None, shape, kind + "_c", store_as=bid)
            return wcache[key]

        misc_bank = {"n": 0}

        def next_misc_bank():
            b = 2 + (misc_bank["n"] % 4)
            misc_bank["n"] += 1
            return b


        def mod_load(hb):
            wblk("w_ada", 0, hb)

        def mod_compute(hb):
            slot, wv = wblk("w_ada", 0, hb)
            bank = next_misc_bank()

            def mm(e):
                out = []
                for jj in range(2):
                    for kc in range(NDC):
                        out.append(e.matmul(ps_all[:, bank, jj * NQ:(jj + 1) * NQ], wv[:, kc, jj * 128:(jj + 1) * 128],
                                            scT[:, kc, :], start=(kc == 0), stop=(kc == NDC - 1)))
                return out
            S.add(PE, mm, reads=[("ring", slot), "scT"], writes=[("ps", bank)], name="mod_mm%d" % hb)

            def ev(e):
                out = []
                for jj in range(2):
                    j = hb * 2 + jj
                    out.append(e.activation(out=modT[:, j, :], in_=ps_all[:, bank, jj * NQ:(jj + 1) * NQ],
                                            func=AF.Identity, bias=vcol(V_BADA + j), scale=1.0))
                return out
            S.add(ACT, ev, reads=[("ps", bank), "vecs"], writes=[("modT", hb)], name="mod_ev%d" % hb)

        def mod_block(hb):
            mod_load(hb)
            mod_compute(hb)

        def mod_keys(which):
            return [("modT", which * 4 + i) for i in range(4)]

        def mk_gsc(which_sc, gvec_off, gsc, name):
            def f(e):
                i1 = e.tensor_scalar(gsc[:], modT[:, which_sc * 8:(which_sc + 1) * 8, :], 1.0, None, ALU.add)
                i2 = e.tensor_tensor(gsc[:], gsc[:], vecs[:, gvec_off:gvec_off + 8].unsqueeze(2).to_broadcast([128, NDC, NQ]), ALU.mult)
                return [i1, i2]
            S.add(DVE, lambda e: [e.tensor_scalar(gsc[:], modT[:, which_sc * 8:(which_sc + 1) * 8, :], 1.0, None, ALU.add)],
                  reads=mod_keys(which_sc), writes=[name + "_t"], name=name + "_a")
            S.add(DVE, lambda e: [e.tensor_tensor(gsc[:], gsc[:], vecs[:, gvec_off:gvec_off + 8].unsqueeze(2).to_broadcast([128, NDC, NQ]), ALU.mult)],
                  reads=[name + "_t", "vecs"], writes=[name], name=name + "_b")

        def CS(sg_):
            return slice(sg_.c0, sg_.c0 + sg_.n)

        def norm1_sq(P):
            sqk = [("zsq", i) for i in range(8)]
            for si, sg_ in enumerate(P.segs):
                S.add(ACT, lambda e, sg_=sg_: [e.activation(out=zsq[:, :, CS(sg_)], in_=xT[:, :, sg_.t0:sg_.t0 + sg_.n], func=AF.Square)],
                      reads=xkeys(P, si), writes=sqk, name="n1_sq%d" % P.idx)

        def norm1(P):
            n = P.n
            sqk = [("zsq", i) for i in range(8)]
            bank = next_misc_bank()
            S.add(PE, lambda e: [e.matmul(PS(bank, n), c1024[:], zsq[:, dc, 0:n], start=(dc == 0), stop=(dc == NDC - 1)) for dc in range(NDC)],
                  reads=sqk + ["c1024"], writes=[("ps", bank)], name="n1_ss%d" % P.idx)
            S.add(ACT, lambda e: [e.activation(out=rstd[:, 0:n], in_=PS(bank, n), func=AF.Ln, bias=EPS, scale=1.0)],
                  reads=[("ps", bank)], writes=["rstd"], name="n1_ln%d" % P.idx)
            S.add(ACT, lambda e: [e.activation(out=rstd[:, 0:n], in_=rstd[:, 0:n], func=AF.Exp, scale=-0.5)], reads=["rstd"], writes=["rstd"],
                  name="n1_exp%d" % P.idx)
        def norm1_dc(P, dc):
            if True:
                r = dc % 2
                for si, sg_ in enumerate(P.segs):
                    cs = CS(sg_)
                    xs = xT[:, dc, sg_.t0:sg_.t0 + sg_.n]
                    if not sg_.sample:
                        S.add(DVE, lambda e, xs=xs, dc=dc, r=r, cs=cs: [e.scalar_tensor_tensor(
                            tmp[:, r, cs], xs, gsc1[:, dc, 0:1], rstd[:, cs], ALU.mult, ALU.mult)],
                            reads=[("x", P.idx, si, dc), "gsc1", "rstd"], writes=[("tmp", r)], name="n1_t")
                        S.add(ACT, lambda e, dc=dc, r=r, cs=cs: [e.activation(
                            out=hT[:, dc, cs], in_=tmp[:, r, cs], func=AF.Identity, bias=mod_scalar(0, dc), scale=1.0)],
                            reads=[("tmp", r)] + mod_keys(0), writes=[("hT", dc)], name="n1_h")
                    else:
                        S.add(DVE, lambda e, xs=xs, r=r, cs=cs: [e.tensor_tensor(tmp[:, r, cs], xs, rstd[:, cs], ALU.mult)],
                              reads=[("x", P.idx, si, dc), "rstd"], writes=[("tmp", r)], name="n1s_a")
                        S.add(DVE, lambda e, dc=dc, r=r, cs=cs: [e.tensor_tensor(
                            v3(tmp[:, r, cs]), v3(tmp[:, r, cs]), bc_seq(gsc1[:, dc, 1:NQ]), ALU.mult)],
                            reads=[("tmp", r), "gsc1"], writes=[("tmp", r)], name="n1s_b")
                        S.add(DVE, lambda e, dc=dc, r=r, cs=cs: [e.tensor_tensor(
                            v3(hT[:, dc, cs]), v3(tmp[:, r, cs]), bc_seq(modT[:, 0 * 8 + dc, 1:NQ]), ALU.add)],
                            reads=[("tmp", r)] + mod_keys(0), writes=[("hT", dc)], name="n1s_c")

        proj_bank = {"n": 0}

        def next_proj_bank():
            b = proj_bank["n"] % 2
            proj_bank["n"] += 1
            return b

        def proj_chunk(P, m):
            n = P.n
            slot, wv = wblk("w_in", P.idx, m // 2)
            j = m % 2
            bank = next_proj_bank()

            def mm(e):
                return [e.matmul(PS(bank, n), wv[:, kc, j * 128:(j + 1) * 128], hT[:, kc, 0:n],
                                 start=(kc == 0), stop=(kc == NDC - 1)) for kc in range(NDC)]
            S.add(PE, mm, reads=[("ring", slot)] + [("hT", dc) for dc in range(NDC)], writes=[("ps", bank)], name="proj_mm")
            return bank

        def in_proj(P, filler=()):
            n = P.n
            filler = list(filler)

            def fill(k):
                for _ in range(k):
                    if filler:
                        filler.pop(0)()
            for c in range(4):
                r = c % 2
                bk = proj_chunk(P, 4 + c)
                S.add(ACT, lambda e, bk=bk, r=r: [e.activation(out=ca[:, r, 0:n], in_=PS(bk, n), func=AF.Copy)],
                      reads=[("ps", bk)], writes=[("ca", r)], name="ca_ev")
                bk2 = proj_chunk(P, 8 + c)
                for sg_ in P.segs:
                    cs = CS(sg_)
                    if not sg_.sample:
                        np_ = sg_.n
                        S.add(DVE, lambda e, bk2=bk2, r=r, c=c, cs=cs, np_=np_: [e.tensor_tensor(ua[:, c, 2:2 + np_], PS(bk2, n)[:, cs], ca[:, r, cs], ALU.mult)],
                              reads=[("ps", bk2), ("ca", r)], writes=[("ua", c)], name="ua_ev")
                        if P.last_prompt:
                            S.add(DVE, lambda e, bk2=bk2, r=r, c=c, np_=np_: [e.tensor_tensor(nap_t[:, c, :], PS(bk2, n)[:, np_ - 2:np_], ca[:, r, np_ - 2:np_], ALU.mult)],
                                  reads=[("ps", bk2), ("ca", r)], writes=["nap_t"], name="nap_ev")
                    else:
                        S.add(DVE, lambda e, bk2=bk2, r=r, c=c, cs=cs: [e.tensor_tensor(ua_s[:, c, :, 2:2 + TS], v3(PS(bk2, n)[:, cs]), v3(ca[:, r, cs]), ALU.mult)],
                              reads=[("ps", bk2), ("ca", r), "ua_s"], writes=[("uas", c)], name="uas_ev")
                        S.add(DVE, lambda e, bk2=bk2, r=r, c=c, cs=cs: [e.tensor_tensor(nas_t[:, c, :, :], v3(PS(bk2, n)[:, cs])[:, :, 2:4], v3(ca[:, r, cs])[:, :, 2:4], ALU.mult)],
                              reads=[("ps", bk2), ("ca", r)], writes=["nas_t"], name="nas_ev")
                fill(2)
            for c in range(4):
                r = c % 2
                bk = proj_chunk(P, 16 + c)
                S.add(ACT, lambda e, bk=bk, r=r: [e.activation(out=sg[:, r, 0:n], in_=PS(bk, n), func=AF.Sigmoid)],
                      reads=[("ps", bk)], writes=[("sg", r)], name="sg_ev")
                bk2 = proj_chunk(P, 12 + c)
                for sg_ in P.segs:
                    cs = CS(sg_)
                    if not sg_.sample:
                        np_ = sg_.n
                        S.add(DVE, lambda e, bk2=bk2, r=r, c=c, cs=cs, np_=np_: [e.tensor_tensor(ub[:, c, 30:30 + np_], PS(bk2, n)[:, cs], sg[:, r, cs], ALU.mult)],
                              reads=[("ps", bk2), ("sg", r)], writes=[("ub", c)], name="ub_ev")
                        if P.last_prompt:
                            S.add(DVE, lambda e, bk2=bk2, r=r, c=c, np_=np_: [e.tensor_tensor(nbp_t[:, c, :], PS(bk2, n)[:, np_ - 30:np_], sg[:, r, np_ - 30:np_], ALU.mult)],
                                  reads=[("ps", bk2), ("sg", r)], writes=["nbp_t"], name="nbp_ev")
                    else:
                        S.add(DVE, lambda e, bk2=bk2, r=r, c=c, cs=cs: [e.tensor_tensor(ub_s[:, c, :, 30:30 + TS], v3(PS(bk2, n)[:, cs]), v3(sg[:, r, cs]), ALU.mult)],
                              reads=[("ps", bk2), ("sg", r), "ub_s"], writes=[("ubs", c)], name="ubs_ev")
                        S.add(DVE, lambda e, bk2=bk2, r=r, c=c, cs=cs: [e.tensor_tensor(nbs_t[:, c, :, 26:30], v3(PS(bk2, n)[:, cs]), v3(sg[:, r, cs]), ALU.mult)],
                              reads=[("ps", bk2), ("sg", r), "nbs_t"], writes=["nbs_t2"], name="nbs_ev")
                fill(3)
            fill(100)
        def in_proj_ba(P):
            n = P.n
            for c in range(4):
                bk = proj_chunk(P, c)
                S.add(ACT, lambda e, bk=bk, c=c: [e.activation(out=ba[:, c, 0:n], in_=PS(bk, n), func=AF.Copy)],
                      reads=[("ps", bk)], writes=[("ba", c)], name="ba_ev")

        def in_proj_gates(P, filler=()):
            n = P.n
            filler = list(filler)
            for half, dst, nm in ((0, sga, "sga"), (1, sgb, "sgb")):
                for m in range(8):
                    bk = proj_chunk(P, 20 + half * 8 + m)
                    S.add(ACT, lambda e, bk=bk, m=m, dst=dst: [e.activation(out=dst[:, m, 0:n], in_=PS(bk, n), func=AF.Sigmoid)],
                          reads=[("ps", bk)], writes=[(nm, m)], name=nm + "_ev")
                    for _ in range(2):
                        if filler:
                            filler.pop(0)()
            while filler:
                filler.pop(0)()

        dg_state = {}

        def gen_diags(P, upto):
            nxt_ = dg_state.get(P.idx, 0)
            upto = min(upto, 4 * 31 - 1)
            while nxt_ <= upto:
                c, k = divmod(nxt_, 31)
                r = nxt_ % NDG
                if nxt_ % 4 == 1:
                    S.add(ACT, lambda e, c=c, k=k, r=r: [e.activation(out=dgb[:, r, :], in_=identB[:], func=AF.Copy, scale=vcol(V_CBW + c * 31 + k))],
                          reads=["identB", "vecs"], writes=[("dgb", r)], name="mk_dgb_act")
                else:
                    S.add(DVE, lambda e, c=c, k=k, r=r: [e.tensor_scalar(dgb[:, r, :], identB[:], vcol(V_CBW + c * 31 + k), None, ALU.mult)],
                          reads=["identB", "vecs"], writes=[("dgb", r)], name="mk_dgb")
                nxt_ += 1
            dg_state[P.idx] = nxt_

        def diag_slot(P, j):
            gen_diags(P, j)
            return j % NDG

        def convs(P, filler=()):
            n = P.n
            filler = list(filler)

            def mv_a(sg_, c, k):
                return ua_s[:, c, :, k:k + TS] if sg_.sample else ua[:, c, k:k + sg_.n]

            def mv_b(sg_, c, k):
                return ub_s[:, c, :, k:k + TS] if sg_.sample else ub[:, c, k:k + sg_.n]

            def outp(sg_, bank):
                return v3(PS(bank, sg_.n)) if sg_.sample else PS(bank, sg_.n)

            for c in range(4):
                banks = [next_misc_bank() for _ in P.segs]

                def mm(e, c=c, banks=banks):
                    out = []
                    for k in range(3):
                        for sg_, bank in zip(P.segs, banks):
                            out.append(e.matmul(outp(sg_, bank), dga[:, c * 3 + k, :], mv_a(sg_, c, k), start=(k == 0), stop=(k == 2)))
                    return out
                S.add(PE, mm, reads=["dga", ("ua", c), ("uas", c), "ua_halo", "ua_s"], writes=[("ps", bk_) for bk_ in banks], name="conva_mm")
                for sg_, bank in zip(P.segs, banks):
                    S.add(DVE, lambda e, c=c, bank=bank, sg_=sg_: [e.tensor_tensor(zsq[:, c, CS(sg_)], PS(bank, sg_.n), ba[:, c, CS(sg_)], ALU.mult)],
                          reads=[("ps", bank), ("ba", c)], writes=[("zsq", c)], name="za_ev")
            if filler:
                filler.pop(0)()
            def stats(c):
                r2 = c % 2
                S.add(PE, lambda e, c=c, r2=r2: [e.matmul(PS(6, n), c512[:], vbf[:, r2, 0:n], start=(c == 0), stop=(c == 3))],
                      reads=[("vbf", r2), "c512"], writes=[("ps", 6)] if c == 0 else [("psMacc", c)], name="ln_mean_mm")
                S.add(PE, lambda e, c=c, r2=r2: [e.matmul(PS(7, n), c512[:], vsq[:, r2, 0:n], start=(c == 0), stop=(c == 3))],
                      reads=[("vsq", r2), "c512"], writes=[("ps", 7)] if c == 0 else [("psEacc", c)], name="ln_ex2_mm")

            for c in range(4):
                banks = [next_misc_bank() for _ in P.segs]
                for k in range(31):
                    r = diag_slot(P, c * 31 + k)

                    def mm(e, c=c, k=k, r=r, banks=banks):
                        return [e.matmul(outp(sg_, bank), dgb[:, r, :], mv_b(sg_, c, k), start=(k == 0), stop=(k == 30))
                                for sg_, bank in zip(P.segs, banks)]
                    S.add(PE, mm, reads=[("dgb", r), ("ub", c), ("ubs", c), "ub_halo", "ub_s"],
                          writes=[("ps", bk_) for bk_ in banks] if k == 0 else [("psCacc", bk_) for bk_ in banks], name="convb_mm")
                    gen_diags(P, c * 31 + k + NDG)
                    if filler and (c * 31 + k) % 14 == 13:
                        filler.pop(0)()
                r2 = c % 2
                for sg_, bank in zip(P.segs, banks):
                    cs = CS(sg_)
                    rk = [("ps", bank), ("psCacc", bank), "vecs"]
                    S.add(ACT, lambda e, c=c, bank=bank, sg_=sg_, cs=cs: [e.activation(out=V(c, n)[:, cs], in_=PS(bank, sg_.n), func=AF.Identity, bias=vcol(V_CBB + c), scale=1.0)],
                          reads=rk, writes=[("v", c)], name="v_ev")
                    S.add(ACT, lambda e, c=c, bank=bank, r2=r2, sg_=sg_, cs=cs: [e.activation(out=vbf[:, r2, cs], in_=PS(bank, sg_.n), func=AF.Identity, bias=vcol(V_CBB + c), scale=1.0)],
                          reads=rk, writes=[("vbf", r2)], name="vbf_ev")
                    S.add(ACT, lambda e, c=c, bank=bank, r2=r2, sg_=sg_, cs=cs: [e.activation(out=vsq[:, r2, cs], in_=PS(bank, sg_.n), func=AF.Square, bias=vcol(V_CBB + c), scale=1.0)],
                          reads=rk, writes=[("vsq", r2)], name="vsq_ev")
                if c >= 1:
                    stats(c - 1)
            stats(3)
            while filler:
                filler.pop(0)()
            if not P.last_prompt:
                np_ = P.segs[0].n
                S.add(POOL, lambda e: [e.tensor_copy(ua[:, :, 0:2], ua[:, :, np_:np_ + 2])],
                      reads=[("ua", c) for c in range(4)], writes=["ua_halo"], name="ua_halo")
                S.add(POOL, lambda e: [e.tensor_copy(ub[:, :, 0:30], ub[:, :, np_:np_ + 30])],
                      reads=[("ub", c) for c in range(4)], writes=["ub_halo"], name="ub_halo")

        def layernorm_b(P):
            n = P.n
            ops = []
            eb = 7
            accs = [("psMacc", c) for c in range(1, 4)]
            accE = [("psEacc", c) for c in range(1, 4)]
            ops.append(lambda: S.add(ACT, lambda e: [e.activation(out=lnM[:, 0:n], in_=PS(6, n), func=AF.Copy)],
                  reads=[("ps", 6)] + accs, writes=["lnM"], name="ln_m"))
            ops.append(lambda: S.add(DVE, lambda e: [e.tensor_tensor(lnB[:, 0:n], lnM[:, 0:n], lnM[:, 0:n], ALU.mult)],
                  reads=["lnM"], writes=["lnB"], name="ln_m2"))
            ops.append(lambda: S.add(DVE, lambda e: [e.tensor_tensor(lnA[:, 0:n], PS(eb, n), lnB[:, 0:n], ALU.subtract)],
                  reads=[("ps", eb), "lnB"] + accE, writes=["lnA"], name="ln_var"))
            ops.append(lambda: S.add(ACT, lambda e: [e.activation(out=lnA[:, 0:n], in_=lnA[:, 0:n], func=AF.Ln, bias=EPS, scale=1.0)],
                  reads=["lnA"], writes=["lnA"], name="ln_ln"))
            ops.append(lambda: S.add(ACT, lambda e: [e.activation(out=lnA[:, 0:n], in_=lnA[:, 0:n], func=AF.Exp, scale=-0.5)],
                  reads=["lnA"], writes=["lnA"], name="ln_exp"))
            ops.append(lambda: S.add(DVE, lambda e: [e.scalar_tensor_tensor(lnB[:, 0:n], lnM[:, 0:n], -1.0, lnA[:, 0:n], ALU.mult, ALU.mult)],
                  reads=["lnM", "lnA"], writes=["lnB"], name="ln_B"))
            for c in range(4):
                ops.append(lambda c=c: S.add(DVE, lambda e, c=c: [e.tensor_tensor(V(c, n), V(c, n), lnA[:, 0:n], ALU.mult)],
                      reads=[("v", c), "lnA"], writes=[("v", c)], name="ln_a"))
                ops.append(lambda c=c: S.add(DVE, lambda e, c=c: [e.tensor_tensor(V(c, n), V(c, n), lnB[:, 0:n], ALU.add)],
                      reads=[("v", c), "lnB"], writes=[("v", c)], name="ln_b"))
                ops.append(lambda c=c: S.add(ACT, lambda e, c=c: [e.activation(out=zsq[:, 4 + c, 0:n], in_=V(c, n), func=AF.Silu,
                                                        bias=vcol(V_LNB + c), scale=vcol(V_LNG + c))],
                      reads=[("v", c), "vecs"], writes=[("zsq", 4 + c)], name="zb_ev"))
            return ops

        def mixer_out(P):
            n = P.n
            for m in range(NDC):
                sa_, wav = wblk("w_oa", P.idx, m // 4)
                sb_, wbv = wblk("w_ob", P.idx, m // 4)
                j = m % 4
                bka = next_misc_bank()
                bkb = next_misc_bank()
                S.add(PE, lambda e, j=j, bka=bka, wav=wav: [e.matmul(PS(bka, n), wav[:, kc, j * 128:(j + 1) * 128], zsq[:, kc, 0:n],
                                                                      start=(kc == 0), stop=(kc == 3)) for kc in range(4)],
                      reads=[("ring", sa_)] + [("zsq", c) for c in range(4)], writes=[("ps", bka)], name="ya_mm")
                S.add(PE, lambda e, j=j, bkb=bkb, wbv=wbv: [e.matmul(PS(bkb, n), wbv[:, kc, j * 128:(j + 1) * 128], zsq[:, 4 + kc, 0:n],
                                                                      start=(kc == 0), stop=(kc == 3)) for kc in range(4)],
                      reads=[("ring", sb_)] + [("zsq", 4 + c) for c in range(4)], writes=[("ps", bkb)], name="yb_mm")
                S.add(DVE, lambda e, m=m, bka=bka: [e.tensor_tensor(mtmp[:, 0, 0:n], PS(bka, n), sga[:, m, 0:n], ALU.mult)],
                      reads=[("ps", bka), ("sga", m)], writes=[("mtmp", 0)], name="mg_a")
                S.add(DVE, lambda e, m=m, bkb=bkb: [e.tensor_tensor(mtmp[:, 1, 0:n], PS(bkb, n), sgb[:, m, 0:n], ALU.mult)],
                      reads=[("ps", bkb), ("sgb", m)], writes=[("mtmp", 1)], name="mg_b")
                S.add(DVE, lambda e, m=m: [e.tensor_tensor(merged[:, m, 0:n], mtmp[:, 0, 0:n], mtmp[:, 1, 0:n], ALU.add)],
                      reads=[("mtmp", 0), ("mtmp", 1)], writes=[("merged", m)], name="mg_sum")

        def out_proj(P):
            n = P.n
            for m in range(NDC):
                so_, wov = wblk("w_o", P.idx, m // 2)
                j = m % 2
                bk = next_misc_bank()
                S.add(PE, lambda e, j=j, bk=bk, wov=wov: [e.matmul(PS(bk, n), wov[:, kc, j * 128:(j + 1) * 128], merged[:, kc, 0:n],
                                                                    start=(kc == 0), stop=(kc == NDC - 1)) for kc in range(NDC)],
                      reads=[("ring", so_)] + [("merged", c) for c in range(NDC)], writes=[("ps", bk)], name="wo_mm")
                for si, sg_ in enumerate(P.segs):
                    cs = CS(sg_)
                    xs = xT[:, m, sg_.t0:sg_.t0 + sg_.n]
                    if not sg_.sample:
                        S.add(DVE, lambda e, m=m, bk=bk, xs=xs, cs=cs: [e.scalar_tensor_tensor(xs, PS(bk, n)[:, cs], mod_scalar(2, m), xs, ALU.mult, ALU.add)],
                              reads=[("ps", bk), ("x", P.idx, si, m)] + mod_keys(2), writes=[("x", P.idx, si, m)], name="x2_ev")
                    else:
                        S.add(DVE, lambda e, m=m, bk=bk, cs=cs: [e.tensor_tensor(v3(mtmp[:, 0, cs]), v3(PS(bk, n)[:, cs]), bc_seq(modT[:, 2 * 8 + m, 1:NQ]), ALU.mult)],
                              reads=[("ps", bk)] + mod_keys(2), writes=[("mtmp", 0)], name="x2s_a")
                        S.add(DVE, lambda e, xs=xs, cs=cs: [e.tensor_tensor(xs, xs, mtmp[:, 0, cs], ALU.add)],
                              reads=[("mtmp", 0), ("x", P.idx, si, m)], writes=[("x", P.idx, si, m)], name="x2s_b")

        norm1_sq(PASSES[0])
        norm1(PASSES[0])
        for hb in range(8):
            mod_block(hb)
        mk_gsc(1, V_N1G, gsc1, "gsc1")
        for dc in range(NDC):
            norm1_dc(PASSES[0], dc)
        in_proj(PASSES[0])
        in_proj_ba(PASSES[0])
        mod_sched = {0: [8, 9, 10, 11], 2: [16, 17, 18, 19], 3: [20, 21, 22, 23], 4: [12, 13, 14, 15]}
        for P in PASSES:
            nxt = PASSES[P.idx + 1] if P.idx + 1 < len(PASSES) else None
            if nxt is not None:
                load_x(nxt, extra_reads=[("hT", 0)])
                norm1_sq(nxt)
            if nxt is not None:
                in_proj_gates(P)
            gen_diags(P, NDG - 1)
            if nxt is not None:
                norm1(nxt)
            mods = mod_sched.get(P.idx, [])
            for hb in mods[:4]:
                mod_load(hb)
            if nxt is not None:
                wblk("w_in", nxt.idx, 2)
                wblk("w_in", nxt.idx, 4)
            convs(P, filler=[(lambda dc=dc: norm1_dc(nxt, dc)) for dc in range(NDC)] if nxt is not None else ())
            ln_ops = layernorm_b(P)
            if nxt is None:
                for hb in mods:
                    mod_compute(hb)
                mods = []
                in_proj_gates(P, filler=ln_ops)
                ln_ops = []
            for hb in mods:
                mod_compute(hb)
            if nxt is not None:
                in_proj(nxt, filler=ln_ops)
            mixer_out(P)
            if nxt is not None:
                in_proj_ba(nxt)
            if P.idx == 4:
                mk_gsc(4, V_N2G, gsc2, "gsc2")
            out_proj(P)

        def out_dma(key, dst, src, reads, name):
            return S.add(SP, lambda e: [e.dma_start(out=dst, in_=src)], reads=reads, writes=[("out", key)], dma=("out", key), ndma=1, name=name)
        out_dma("nap", nap_d[:, :], nap_t[:].rearrange("p a b -> p (a b)"), ["nap_t"], "st_nap")
        out_dma("nbp", nbp_d[:, :], nbp_t[:].rearrange("p a b -> p (a b)"), ["nbp_t"], "st_nbp")
        out_dma("nas", nas_d[:, :], nas_t[:].rearrange("p a b c -> p (a b c)"), ["nas_t"], "st_nas")
        out_dma("nbs", nbs_d[:, :], nbs_t[:].rearrange("p a b c -> p (a b c)"), ["nbs_t", "nbs_t2"], "st_nbs")

        all_keys = list(S.last_w.keys())
        S.add(SP, lambda e: [], writes=all_keys + ["__bar__"], name="barrier_sp")
        for eng in (PE, ACT, DVE, POOL):
            S.add(eng, lambda e: [], reads=["__bar__"], name="fence_" + eng)
        esA.__exit__(None, None, None)

        def sbB(name, shape, dt):
            return es.enter_context(nc.sbuf_tensor(name, list(shape), dt))

        h2T = sbB("h2T", [128, NDC, NT], BF16)
        h2f = sbB("h2f", [128, NDC, 512], F32)
        sqB = sbB("sqB", [128, NDC, 512], BF16)
        rstdB = sbB("rstdB", [128, 512], F32)
        wr_sb = sbB("wr_sb", [128, NDC, 20], F32)
        LT = sbB("LT", [20, 512], F32)
        NTT = 17
        L = sbB("L", [128, NTT, 20], F32)
        gmax = sbB("gmax", [128, NTT], F32)
        gmask = sbB("gmask", [128, NTT, 4], F32)
        gsh = sbB("gsh", [128, NTT, 4], F32)
        gsum = sbB("gsum", [128, NTT], F32)
        pg = sbB("pg", [128, NTT], F32)
        pen = sbB("pen", [128, NTT, 4], F32)
        Em = sbB("Em", [128, NTT, 16], F32)
        Em2 = Em
        m1 = sbB("m1", [128, NTT, 16], F32)
        m2 = sbB("m2", [128, NTT, 16], F32)
        v1 = sbB("v1", [128, NTT], F32)
        v2 = sbB("v2", [128, NTT], F32)
        dd = sbB("dd", [128, NTT], F32)
        w1s = sbB("w1s", [128, NTT], F32)
        w2s = sbB("w2s", [128, NTT], F32)
        comb = m1
        combT = sbB("combT", [16, NT], BF16)
        sel = sbB("sel", [16, 16, 128], BF16)
        NWS = 2
        wA = [sbB("wA%d" % i, [128, 2, NDC, DE], BF16) for i in range(NWS)]
        wB = [sbB("wB%d" % i, [128, 2, NDC, DE], BF16) for i in range(NWS)]
        wC = [sbB("wC%d" % i, [128, 2, 2, D], BF16) for i in range(NWS)]
        abuf = [sqB[:, 0:4, :], sqB[:, 4:8, :]]
        slb = sbB("slb", [128, 2, 512], F32)
        tmpB = slb
        tlb = sbB("tlb", [128, 2, 512], F32)
        gtmp = sbB("gtmp", [128, 512], F32)

        FK = []

        dma(SP, "ld_wr", wr_sb[:].rearrange("p a b -> p (a b)"), wr_d[:, :], reads=FK, writes=["wr_sb"], name="ld_wr")

        def load_pair(p):
            slot = p % NWS
            for el in range(2):
                e_ = 2 * p + el
                S.add(POOL, lambda e, e_=e_, el=el, slot=slot: [
                    e.dma_start(out=wA[slot][:, el, :, :], in_=w1_d[e_].rearrange("(kc p) f -> p kc f", p=128)),
                    e.dma_start(out=wB[slot][:, el, :, :], in_=w3_d[e_].rearrange("(kc p) f -> p kc f", p=128)),
                    e.dma_start(out=wC[slot][:, el, :, :], in_=w2_d[e_].rearrange("(fc p) d -> p fc d", p=128))],
                    reads=FK, writes=[("wexp", slot, el)], dma=("wexp", slot, el), ndma=3, name="ld_pair%d_%d" % (p, el))

        load_pair(0)
        load_pair(1)

        S.add(DVE, lambda e: [e.memset(L[:], 0.0)], reads=FK, writes=["L"], name="L0")
        S.add(DVE, lambda e: [e.tensor_copy(sel[:], identF[0:16, 0:16].unsqueeze(2).to_broadcast([16, 16, 128]))],
              reads=FK + ["identF"], writes=["sel"], name="mk_sel")

        rbufs = [rstdB, gtmp]
        rkeys = ["rstdB", "gtmp"]

        def n2_A(st):
            n = st.n
            rb, rk = rbufs[st.idx % 2], rkeys[st.idx % 2]
            sqk = [("h2T", st.idx, i) for i in range(8)]
            S.add(ACT, lambda e: [e.activation(out=h2T[:, :, st.t0:st.t0 + n], in_=xT[:, :, st.t0:st.t0 + n], func=AF.Square)],
                  reads=[("x", st.idx, dc) for dc in range(NDC)], writes=sqk, name="n2_sq")

        def n2_A2(st):
            n = st.n
            rb, rk = rbufs[st.idx % 2], rkeys[st.idx % 2]
            sqk = [("h2T", st.idx, i) for i in range(8)]
            sbank = st.idx % 2
            S.add(PE, lambda e: [e.matmul(PS(sbank, n), c1024[:], h2T[:, dc, st.t0:st.t0 + n], start=(dc == 0), stop=(dc == NDC - 1)) for dc in range(NDC)],
                  reads=sqk + ["c1024"], writes=[("ps", sbank)], name="n2_ss")
            S.add(ACT, lambda e: [e.activation(out=rb[:, 0:n], in_=PS(sbank, n), func=AF.Ln, bias=EPS, scale=1.0)],
                  reads=[("ps", sbank)], writes=[rk], name="n2_ln")
            S.add(ACT, lambda e: [e.activation(out=rb[:, 0:n], in_=rb[:, 0:n], func=AF.Exp, scale=-0.5)], reads=[rk], writes=[rk], name="n2_exp")

        def n2_B(st):
            n = st.n
            npr = st.npr
            rb, rk = rbufs[st.idx % 2], rkeys[st.idx % 2]
            for dc in range(NDC):
                xs = xT[:, dc, st.t0:st.t0 + npr]
                S.add(DVE, lambda e, xs=xs, dc=dc: [e.scalar_tensor_tensor(
                    h2f[:, dc, 0:npr], xs, gsc2[:, dc, 0:1], rb[:, 0:npr], ALU.mult, ALU.mult)],
                    reads=[("x", st.idx, dc), "gsc2", rk], writes=[("h2f", dc)], name="n2_t")
            for dc in range(NDC):
                S.add(ACT, lambda e, dc=dc: [e.activation(
                    out=h2f[:, dc, 0:npr], in_=h2f[:, dc, 0:npr], func=AF.Identity, bias=mod_scalar(3, dc), scale=1.0)],
                    reads=[("h2f", dc)] + mod_keys(3), writes=[("h2f", dc)], name="n2_h")
            if npr < n:
                for dc in range(NDC):
                    r = dc % 2
                    xs = xT[:, dc, st.t0 + npr:st.t0 + n]
                    S.add(DVE, lambda e, xs=xs, r=r: [e.tensor_tensor(tmpB[:, r, npr:n], xs, rb[:, npr:n], ALU.mult)],
                          reads=[("x", st.idx, dc), rk], writes=[("slb", r)], name="n2s_a")
                    S.add(DVE, lambda e, dc=dc, r=r: [e.tensor_tensor(
                        v3(tmpB[:, r, npr:n]), v3(tmpB[:, r, npr:n]), bc_seq(gsc2[:, dc, 1:NQ]), ALU.mult)],
                        reads=[("slb", r), "gsc2"], writes=[("slb", r)], name="n2s_b")
                    S.add(DVE, lambda e, dc=dc, r=r: [e.tensor_tensor(
                        v3(h2f[:, dc, npr:n]), v3(tmpB[:, r, npr:n]), bc_seq(modT[:, 3 * 8 + dc, 1:NQ]), ALU.add)],
                        reads=[("slb", r), ] + mod_keys(3), writes=[("h2f", dc)], name="n2s_c")
            for dc in range(NDC):
                S.add(DVE, lambda e, dc=dc: [e.tensor_copy(h2T[:, dc, st.t0:st.t0 + n], h2f[:, dc, 0:n])],
                      reads=[("h2f", dc)], writes=[("h2T", st.idx, dc)], name="h2T_cast")

        def n2_C(st):
            n = st.n
            lbank = 2 + (st.idx % 2)
            S.add(PE, lambda e, lbank=lbank: [e.matmul(ps_all[0:20, lbank, 0:n], wr_sb[:, kc, :], h2f[:, kc, 0:n],
                                                       start=(kc == 0), stop=(kc == NDC - 1)) for kc in range(NDC)],
                  reads=[("h2f", dc) for dc in range(NDC)] + ["wr_sb"], writes=[("ps", lbank)], name="router_mm")
            S.add(ACT, lambda e, lbank=lbank: [e.activation(out=LT[:, 0:n], in_=ps_all[0:20, lbank, 0:n], func=AF.Copy)],
                  reads=[("ps", lbank)], writes=["LT"], name="LT_ev")
            ntile = (n + 127) // 128
            for ti in range(ntile):
                rows = min(128, n - ti * 128)
                gt = st.t0 // 128 + ti
                tb = 4 + (gt % 4)
                S.add(PE, lambda e, ti=ti, rows=rows, tb=tb: [e.matmul(ps_all[0:rows, tb, 0:20], LT[:, ti * 128:ti * 128 + rows],
                                                                        identF[0:20, 0:20], start=True, stop=True)],
                      reads=["LT", "identF"], writes=[("ps", tb)], name="L_tr")
                S.add(DVE, lambda e, rows=rows, gt=gt, tb=tb: [e.tensor_tensor(L[0:rows, gt, :], ps_all[0:rows, tb, 0:20], vecs[0:rows, V_BR:V_BR + 20], ALU.add)],
                      reads=[("ps", tb), "L", "vecs"], writes=[("L", gt)], name="L_ev")

        def route(st):
            g0 = st.t0 // 128
            nt = (st.n + 127) // 128
            g1 = g0 + nt
            Lk = [("L", g) for g in range(g0, g1)] + ["L"]
            G = L[:, g0:g1, 0:4]
            E4 = L[:, g0:g1, 4:20].rearrange("p t (g j) -> p t g j", g=4)
            K = lambda nm: ("rt", nm, st.idx)

            ops = []

            def dv(fn, reads, writes, name):
                ops.append(lambda: S.add(DVE, lambda e: [fn(e)], reads=reads, writes=writes, name=name))

            def b3(ap2, k):
                return ap2.unsqueeze(2).to_broadcast([128, nt, k])

            gm, gk, gs, gu, pgs = gmax[:, g0:g1], gmask[:, g0:g1, :], gsh[:, g0:g1, :], gsum[:, g0:g1], pg[:, g0:g1]
            pn, em, mm1, mm2 = pen[:, g0:g1, :], Em[:, g0:g1, :], m1[:, g0:g1, :], m2[:, g0:g1, :]
            vv1, vv2, dds, ww1, ww2 = v1[:, g0:g1], v2[:, g0:g1], dd[:, g0:g1], w1s[:, g0:g1], w2s[:, g0:g1]
            dv(lambda e: e.tensor_reduce(gm, G, AX.X, ALU.max), Lk, [K("gmax")], "r_gmax")
            dv(lambda e: e.tensor_tensor(gk, G, b3(gm, 4), ALU.is_equal), Lk + [K("gmax")], [K("gmask")], "r_gmask")
            dv(lambda e: e.tensor_tensor(gs, G, b3(gm, 4), ALU.subtract), Lk + [K("gmax")], [K("gsh")], "r_gsh")
            ops.append(lambda: S.add(ACT, lambda e: [e.activation(out=gs, in_=gs, func=AF.Exp)], reads=[K("gsh")], writes=[K("gsh")], name="r_gexp"))
            dv(lambda e: e.tensor_reduce(gu, gs, AX.X, ALU.add), [K("gsh")], [K("gsum")], "r_gsum")
            dv(lambda e: e.reciprocal(pgs, gu), [K("gsum")], [K("pg")], "r_pg")
            dv(lambda e: e.tensor_scalar(pn, gk, BIG, -BIG, ALU.mult, ALU.add), [K("gmask")], [K("pen")], "r_pen")
            dv(lambda e: e.tensor_tensor(em.rearrange("p t (g j) -> p t g j", g=4), E4,
                                         pn.unsqueeze(3).to_broadcast([128, nt, 4, 4]), ALU.add), Lk + [K("pen")], [K("Em")], "r_Em")
            dv(lambda e: e.tensor_reduce(vv1, em, AX.X, ALU.max), [K("Em")], [K("v1")], "r_v1")
            dv(lambda e: e.tensor_tensor(mm1, em, b3(vv1, 16), ALU.is_equal), [K("Em"), K("v1")], [K("m1")], "r_m1")
            dv(lambda e: e.scalar_tensor_tensor(em, mm1, -BIG, em, ALU.mult, ALU.add), [K("m1"), K("Em")], [K("Em")], "r_Em2")
            dv(lambda e: e.tensor_reduce(vv2, em, AX.X, ALU.max), [K("Em")], [K("v2")], "r_v2")
            dv(lambda e: e.tensor_tensor(mm2, em, b3(vv2, 16), ALU.is_equal), [K("Em"), K("v2")], [K("m2")], "r_m2")
            dv(lambda e: e.tensor_tensor(dds, vv2, vv1, ALU.subtract), [K("v1"), K("v2")], [K("dd")], "r_dd")
            ops.append(lambda: S.add(ACT, lambda e: [e.activation(out=ww2, in_=dds, func=AF.Sigmoid)], reads=[K("dd")], writes=[K("w2s")], name="r_w2s"))
            dv(lambda e: e.tensor_scalar(ww1, ww2, -1.0, 1.0, ALU.mult, ALU.add), [K("w2s")], [K("w1s")], "r_w1s")
            dv(lambda e: e.tensor_tensor(ww1, ww1, pgs, ALU.mult), [K("w1s"), K("pg")], [K("w1s")], "r_w1p")
            dv(lambda e: e.tensor_tensor(ww2, ww2, pgs, ALU.mult), [K("w2s"), K("pg")], [K("w2s")], "r_w2p")
            dv(lambda e: e.tensor_tensor(mm1, mm1, b3(ww1, 16), ALU.mult), [K("m1"), K("w1s")], [K("m1")], "r_c1")
            dv(lambda e: e.tensor_tensor(mm2, mm2, b3(ww2, 16), ALU.mult), [K("m2"), K("w2s")], [K("m2")], "r_c2")
            dv(lambda e: e.tensor_tensor(mm1, mm1, mm2, ALU.add), [K("m1"), K("m2")], [K("m1"), K("comb")], "r_comb")
            return ops

        def comb_T(st):
            n = st.n
            ntile = (n + 127) // 128
            bank = 4 + (cb_state["n"] % 2)
            cb_state["n"] += 1

            def mm(e):
                out = []
                for ti in range(ntile):
                    rows = min(128, st.n - ti * 128)
                    gt = st.t0 // 128 + ti
                    out.append(e.matmul(ps_all[0:16, bank, ti * 128:ti * 128 + rows], comb[0:rows, gt, :], identF[0:rows, 0:rows], start=True, stop=True))
                return out
            S.add(PE, mm, reads=[("rt", "comb", st.idx), "identF"], writes=[("ps", bank)], name="combT_mm")
            S.add(ACT, lambda e: [e.activation(out=combT[:, st.t0:st.t0 + st.n], in_=ps_all[0:16, bank, 0:st.n], func=AF.Copy)],
                  reads=[("ps", bank)], writes=[("combT", st.idx)], name="combT_ev")

        cb_state = {"n": 0}

        hb_state = {"n": 0}
        yb_state = {"n": 0}

        def moe_H(p, st, par):
            n = st.n
            slot = p % NWS
            ab = abuf[par]
            for el in range(2):
                e_ = 2 * p + el
                cbk = 4 + (cb_state["n"] % 2)
                cb_state["n"] += 1
                S.add(PE, lambda e, e_=e_, cbk=cbk: [e.matmul(PS(cbk, n), sel[:, e_, :], combT[:, st.t0:st.t0 + n], start=True, stop=True)],
                      reads=["sel", ("combT", st.idx)], writes=[("ps", cbk)], name="comb_bc_mm")
                for f in range(2):
                    hb = (hb_state["n"] % 2) * 2
                    hb_state["n"] += 1
                    S.add(PE, lambda e, el=el, f=f, hb=hb: [e.matmul(PS(hb, n), wA[slot][:, el, kc, f * 128:(f + 1) * 128], h2T[:, kc, st.t0:st.t0 + n],
                                                                     start=(kc == 0), stop=(kc == NDC - 1)) for kc in range(NDC)],
                          reads=[("wexp", slot, el)] + [("h2T", st.idx, dc) for dc in range(NDC)], writes=[("ps", hb)], name="h1_mm")
                    S.add(PE, lambda e, el=el, f=f, hb=hb: [e.matmul(PS(hb + 1, n), wB[slot][:, el, kc, f * 128:(f + 1) * 128], h2T[:, kc, st.t0:st.t0 + n],
                                                                     start=(kc == 0), stop=(kc == NDC - 1)) for kc in range(NDC)],
                          reads=[("wexp", slot, el)] + [("h2T", st.idx, dc) for dc in range(NDC)], writes=[("ps", hb + 1)], name="h3_mm")
                    r = f
                    S.add(ACT, lambda e, hb=hb, r=r: [e.activation(out=slb[:, r, 0:n], in_=PS(hb, n), func=AF.Silu)],
                          reads=[("ps", hb)], writes=[("slb", r)], name="silu_ev")
                    S.add(DVE, lambda e, hb=hb, r=r: [e.tensor_tensor(tlb[:, r, 0:n], PS(hb + 1, n), slb[:, r, 0:n], ALU.mult)],
                          reads=[("ps", hb + 1), ("slb", r)], writes=[("tlb", r)], name="a_ev1")
                    S.add(DVE, lambda e, cbk=cbk, r=r, el=el, f=f: [e.tensor_tensor(ab[:, el * 2 + f, 0:n], PS(cbk, n), tlb[:, r, 0:n], ALU.mult)],
                          reads=[("ps", cbk), ("tlb", r)], writes=[("sqB", par * 4 + el * 2 + f)], name="a_ev2")

        def moe_Y(p, st, par, filler=()):
            n = st.n
            slot = p % NWS
            ab = abuf[par]
            filler = list(filler)
            for m in range(NDC):
                for _ in range(3):
                    if filler:
                        filler.pop(0)()
                yb = 6 + (yb_state["n"] % 2)
                yb_state["n"] += 1

                def mm(e, m=m, yb=yb):
                    out = []
                    i = 0
                    for el in range(2):
                        for f in range(2):
                            out.append(e.matmul(PS(yb, n), wC[slot][:, el, f, m * 128:(m + 1) * 128], ab[:, el * 2 + f, 0:n], start=(i == 0), stop=(i == 3)))
                            i += 1
                    return out
                S.add(PE, mm, reads=[("wexp", slot, 0), ("wexp", slot, 1)] + [("sqB", par * 4 + i) for i in range(4)],
                      writes=[("ps", yb)], name="y_mm")
                npr = st.npr
                xs = xT[:, m, st.t0:st.t0 + npr]
                S.add(DVE, lambda e, m=m, yb=yb, xs=xs, npr=npr: [e.scalar_tensor_tensor(xs, PS(yb, n)[:, 0:npr], mod_scalar(5, m), xs, ALU.mult, ALU.add)],
                      reads=[("ps", yb), ("x", st.idx, m), ] + mod_keys(5), writes=[("x", st.idx, m)], name="x3_ev")
                if npr < n:
                    xs2 = xT[:, m, st.t0 + npr:st.t0 + n]
                    S.add(DVE, lambda e, m=m, yb=yb, npr=npr: [e.tensor_tensor(v3(gtmp[:, npr:n]), v3(PS(yb, n)[:, npr:n]), bc_seq(modT[:, 5 * 8 + m, 1:NQ]), ALU.mult)],
                          reads=[("ps", yb), ] + mod_keys(5), writes=["gtmp"], name="x3s_a")
                    S.add(DVE, lambda e, xs2=xs2, npr=npr: [e.tensor_tensor(xs2, xs2, gtmp[:, npr:n], ALU.add)],
                          reads=["gtmp", ("x", st.idx, m)], writes=[("x", st.idx, m)], name="x3s_b")

            while filler:
                filler.pop(0)()

        sqC = h2f[:].bitcast(BF16)

        def final_norm(st):
            n = st.n
            sqk = [("h2f", i) for i in range(8)]
            S.add(ACT, lambda e: [e.activation(out=sqC[:, :, 0:n], in_=xT[:, :, st.t0:st.t0 + n], func=AF.Square)],
                  reads=[("x", st.idx, dc) for dc in range(NDC)], writes=sqk, name="n3_sq")
            bank = 4 + (cb_state["n"] % 2)
            cb_state["n"] += 1
            S.add(PE, lambda e: [e.matmul(PS(bank, n), c1024[:], sqC[:, dc, 0:n], start=(dc == 0), stop=(dc == NDC - 1)) for dc in range(NDC)],
                  reads=sqk + ["c1024"], writes=[("ps", bank)], name="n3_ss")
            S.add(ACT, lambda e: [e.activation(out=rstdB[:, 0:n], in_=PS(bank, n), func=AF.Ln, bias=EPS, scale=1.0)],
                  reads=[("ps", bank)], writes=["rstdB"], name="n3_ln")
            S.add(ACT, lambda e: [e.activation(out=rstdB[:, 0:n], in_=rstdB[:, 0:n], func=AF.Exp, scale=-0.5)], reads=["rstdB"], writes=["rstdB"], name="n3_exp")
            for dc in range(NDC):
                xs = xT[:, dc, st.t0:st.t0 + n]
                S.add(DVE, lambda e, xs=xs, dc=dc: [e.scalar_tensor_tensor(xs, xs, vcol(V_FNG + dc), rstdB[:, 0:n], ALU.mult, ALU.mult)],
                      reads=[("x", st.idx, dc), "vecs", "rstdB"], writes=[("x", st.idx, dc)], name="n3_y")
            S.add(SP, lambda e: [e.dma_start(out=yT_dv[:, :, st.t0:st.t0 + n], in_=xT[:, :, st.t0:st.t0 + n])],
                  reads=[("x", st.idx, dc) for dc in range(NDC)], writes=[("out", "y", st.idx)], dma=("out", "y", st.idx), ndma=1, name="st_y")

        NP = NE // 2
        NS = len(SUPERTILES)
        ST_ = SUPERTILES
        last_order = [ST_[4], ST_[1], ST_[2], ST_[3], ST_[0]]
        seqB = [(p, st) for p in range(NP - 1) for st in SUPERTILES] + [(NP - 1, st) for st in last_order]
        n2_A(ST_[0])
        n2_A2(ST_[0])
        n2_A(ST_[1])
        n2_B(ST_[0])
        n2_A2(ST_[1])
        n2_C(ST_[0])
        for op_ in route(ST_[0]):
            op_()
        comb_T(ST_[0])
        n2_B(ST_[1])
        moe_H(0, ST_[0], 0)
        for i, (p, st) in enumerate(seqB):
            if i + 1 < len(seqB):
                p1, st1 = seqB[i + 1]
                if p1 == 0:
                    if st1.idx + 1 < NS:
                        n2_A(ST_[st1.idx + 1])
                    n2_C(st1)
                    moe_Y(p, st, i % 2, filler=route(st1))
                    if st1.idx + 1 < NS:
                        n2_A2(ST_[st1.idx + 1])
                    comb_T(st1)
                    if st1.idx + 1 < NS:
                        n2_B(ST_[st1.idx + 1])
                    moe_H(p1, st1, (i + 1) % 2)
                else:
                    moe_H(p1, st1, (i + 1) % 2)
                    moe_Y(p, st, i % 2)
            else:
                moe_Y(p, st, i % 2)
            if p == NP - 1:
                j = i - (NP - 1) * NS
                if j >= 2:
                    final_norm(last_order[j - 2])
                if j == NS - 1:
                    final_norm(last_order[j - 1])
                    final_norm(st)
            if i % NS == NS - 1 and p + 2 < NP:
                load_pair(p + 2)

        S.add(SP, lambda e: [], reads=[k for k in S.last_w.keys() if isinstance(k, tuple) and k[0] == "out"], writes=["__done__"], name="final_wait")

        S.finalize()
        eng_sems = {eng: es.enter_context(nc.semaphore("sem_" + eng)) for eng in ENGINES}
        dma_sems = {}
        for i, key in enumerate(S.dma_count.keys()):
            dma_sems[key] = es.enter_context(nc.semaphore("dsem%d" % i))
        block = es.enter_context(nc.Block())

        @block.tensor
        def _(e):
            S.emit_engine(PE, e, eng_sems, dma_sems)

        @block.scalar
        def _(e):
            S.emit_engine(ACT, e, eng_sems, dma_sems)

        @block.vector
        def _(e):
            S.emit_engine(DVE, e, eng_sems, dma_sems)

        @block.gpsimd
        def _(e):
            S.emit_engine(POOL, e, eng_sems, dma_sems)

        @block.sync
        def _(e):
            S.emit_engine(SP, e, eng_sems, dma_sems)

    return nc


_NC_CACHE = {}


def _prep_inputs(inputs):
    f32 = np.float32
    g = lambda k: np.asarray(inputs[k], dtype=f32)
    x_prompt, x_sample = g("x_prompt"), g("x_sample")
    c_prompt, c_sample = g("c_prompt"), g("c_sample")
    sca, scb = g("state_conv_a")[0], g("state_conv_b")[0]

    vecs = np.zeros((128, NV), f32)
    vecs[:, V_BADA:V_BADA + 48] = g("b_ada")[0].reshape(48, 128).T
    vecs[:, V_N1G:V_N1G + 8] = g("norm1_g")[0].reshape(8, 128).T
    vecs[:, V_N2G:V_N2G + 8] = g("norm2_g")[0].reshape(8, 128).T
    vecs[:, V_FNG:V_FNG + 8] = g("final_norm_g").reshape(8, 128).T
    caw = g("conv_a_w")[0]
    vecs[:, V_CAW:V_CAW + 12] = caw.reshape(3, 4, 128).transpose(2, 1, 0).reshape(128, 12)
    cbw = g("conv_b_w")[0]
    vecs[:, V_CBW:V_CBW + 124] = cbw.reshape(31, 4, 128).transpose(2, 1, 0).reshape(128, 124)
    vecs[:, V_CBB:V_CBB + 4] = g("conv_b_bias")[0].reshape(4, 128).T
    vecs[:, V_LNG:V_LNG + 4] = g("ln_b_g")[0].reshape(4, 128).T
    vecs[:, V_LNB:V_LNB + 4] = g("ln_b_b")[0].reshape(4, 128).T
    br = np.concatenate([g("b_group")[0], g("b_expert")[0]])
    vecs[:, V_BR:V_BR + 20] = br[None, :]
    wr = np.concatenate([g("w_group")[0], g("w_expert")[0]], axis=1)
    wr_l = np.ascontiguousarray(wr.reshape(8, 128, 20).transpose(1, 0, 2).reshape(128, 160))
    shared = {
        "vecs": vecs, "ident": np.eye(128, dtype=f32),
        "w_ada": g("w_ada")[0], "w_in": g("w_in")[0], "w_out_a": g("w_out_a")[0], "w_out_b": g("w_out_b")[0],
        "w_o": g("w_o")[0], "wr": wr_l, "w1": g("w1")[0], "w3": g("w3")[0], "w2": g("w2")[0],
    }
    in_maps = []
    for i in range(NCORES):
        xs = x_sample[16 * i:16 * i + 16].reshape(64, D)
        xt = np.concatenate([x_prompt[i], xs], axis=0)
        xT = np.ascontiguousarray(xt.reshape(NT, 8, 128).transpose(2, 1, 0).reshape(128, 8 * NT))
        c17 = np.concatenate([c_prompt[i:i + 1], c_sample[16 * i:16 * i + 16]], axis=0)
        cT = np.ascontiguousarray(c17.reshape(NQ, 8, 128).transpose(2, 1, 0).reshape(128, 8 * NQ))
        sa = sca[16 * i:16 * i + 16]
        sa_l = np.ascontiguousarray(sa.reshape(16, 2, 4, 128).transpose(3, 2, 0, 1).reshape(128, 4 * 16 * 2))
        sbb = scb[16 * i:16 * i + 16]
        sb_l = np.ascontiguousarray(sbb.reshape(16, 30, 4, 128).transpose(3, 2, 0, 1).reshape(128, 4 * 16 * 30))
        m = dict(shared)
        m.update({"xT": xT, "cT": cT, "sa": sa_l, "sb": sb_l})
        in_maps.append(m)
    return in_maps


def kernel(**inputs):
    if "nc" not in _NC_CACHE:
        _NC_CACHE["nc"] = build_program()
    nc = _NC_CACHE["nc"]
    in_maps = _prep_inputs(inputs)
    res = run_bass_kernel_spmd(nc, in_maps, core_ids=list(range(NCORES)))
    f32 = np.float32
    y_prompt = np.empty((8, SEQ, D), f32)
    y_sample = np.empty((128, TS, D), f32)
    nap = np.empty((1, 8, 2, 512), f32)
    nbp = np.empty((1, 8, 30, 512), f32)
    nas = np.empty((1, 128, 2, 512), f32)
    nbs = np.empty((1, 128, 30, 512), f32)
    for i in range(NCORES):
        r = res.results[i]
        yT = np.asarray(r["yT"], dtype=f32).reshape(128, 8, NT)
        yt = yT.transpose(2, 1, 0).reshape(NT, D)
        y_prompt[i] = yt[:SEQ]
        y_sample[16 * i:16 * i + 16] = yt[SEQ:].reshape(16, TS, D)
        nap[0, i] = np.asarray(r["nap"], f32).reshape(128, 4, 2).transpose(2, 1, 0).reshape(2, 512)
        nbp[0, i] = np.asarray(r["nbp"], f32).reshape(128, 4, 30).transpose(2, 1, 0).reshape(30, 512)
        nas[0, 16 * i:16 * i + 16] = np.asarray(r["nas"], f32).reshape(128, 4, 16, 2).transpose(2, 3, 1, 0).reshape(16, 2, 512)
        nbs[0, 16 * i:16 * i + 16] = np.asarray(r["nbs"], f32).reshape(128, 4, 16, 30).transpose(2, 3, 1, 0).reshape(16, 30, 512)
    return (y_prompt, y_sample, nap, nbp, nas, nbs)
```

```python
from contextlib import ExitStack

import numpy as np

import concourse.bass as bass
import concourse.mybir as mybir
from concourse.bass_utils import run_bass_kernel_spmd

F32 = mybir.dt.float32
BF16 = mybir.dt.bfloat16
AF = mybir.ActivationFunctionType
ALU = mybir.AluOpType
AX = mybir.AxisListType

PE, ACT, DVE, POOL, SP = "pe", "act", "dve", "pool", "sp"
ENGINES = (PE, ACT, DVE, POOL, SP)

NCORES = 8
D = 1024
NDC = 8
SEQ = 2048
NSEQ_S = 16
TS = 4
NT = SEQ + NSEQ_S * TS
NQ = 1 + NSEQ_S
D_IN = 4608
NE = 16
DE = 256
EPS = 1e-6
BIG = 1.0e30

V_BADA = 0
V_N1G = 48
V_N2G = 56
V_FNG = 64
V_CAW = 72
V_CBW = 84
V_CBB = 208
V_LNG = 212
V_LNB = 216
V_BR = 220
NV = 240


class Op:
    __slots__ = ("eng", "fn", "deps", "pos", "is_dma", "ndma", "semkey", "needs_inc", "seq", "target", "name")


class Sched:
    def __init__(self):
        self.prog = {e: [] for e in ENGINES}
        self.last_w = {}
        self.readers = {}
        self.dma_count = {}

    def add(self, eng, fn, reads=(), writes=(), dma=None, ndma=1, name=""):
        op = Op()
        op.eng, op.fn, op.name = eng, fn, name
        op.is_dma = dma is not None
        op.semkey, op.ndma = dma, ndma
        deps = set()
        for k in reads:
            w = self.last_w.get(k)
            if w is not None:
                deps.add(w)
        for k in writes:
            w = self.last_w.get(k)
            if w is not None:
                deps.add(w)
            for r in self.readers.get(k, ()):
                deps.add(r)
        for k in reads:
            self.readers.setdefault(k, []).append(op)
        for k in writes:
            self.last_w[k] = op
            self.readers[k] = []
        deps.discard(op)
        op.deps = deps
        op.pos = len(self.prog[eng])
        op.needs_inc = False
        op.seq = None
        op.target = None
        if op.is_dma:
            c = self.dma_count.get(dma, 0) + 16 * ndma
            self.dma_count[dma] = c
            op.target = c
        self.prog[eng].append(op)
        return op

    @staticmethod
    def needs_wait(op, d):
        if d.is_dma:
            return True
        if d.eng == op.eng:
            if d.eng == PE:
                return False
            return True
        return True

    def finalize(self):
        for eng in ENGINES:
            for op in self.prog[eng]:
                for d in op.deps:
                    if (not d.is_dma) and self.needs_wait(op, d):
                        d.needs_inc = True
        for eng in ENGINES:
            c = 0
            for op in self.prog[eng]:
                if op.needs_inc and not op.is_dma:
                    c += 1
                    op.seq = c

    def emit_engine(self, eng, engobj, eng_sems, dma_sems):
        waited = {}
        for op in self.prog[eng]:
            need = {}
            for d in op.deps:
                if d.is_dma:
                    key, val = ("dma", d.semkey), d.target
                elif self.needs_wait(op, d):
                    key, val = ("eng", d.eng), d.seq
                else:
                    continue
                if need.get(key, 0) < val:
                    need[key] = val
            for key, val in need.items():
                if waited.get(key, 0) < val:
                    sem = dma_sems[key[1]] if key[0] == "dma" else eng_sems[key[1]]
                    engobj.wait_ge(sem, val)
                    waited[key] = val
            insts = op.fn(engobj)
            if insts is None:
                insts = []
            elif not isinstance(insts, (list, tuple)):
                insts = [insts]
            if op.is_dma:
                assert len(insts) == op.ndma, (op.name, len(insts), op.ndma)
                for i in insts:
                    i.then_inc(dma_sems[op.semkey], 16)
            elif op.needs_inc:
                if len(insts) == 0:
                    insts = [engobj.nop(nofuse=True)]
                insts[-1].then_inc(eng_sems[eng], 1)


class ST:
    def __init__(self, idx, t0, n, npr):
        self.idx, self.t0, self.n, self.npr = idx, t0, n, npr


class Seg:
    def __init__(self, kind, c0, n, t0):
        self.kind, self.c0, self.n, self.t0 = kind, c0, n, t0
        self.sample = (kind == "s")


class Pass:
    def __init__(self, idx, segs, last_prompt):
        self.idx, self.segs, self.last_prompt = idx, segs, last_prompt
        self.n = sum(sg_.n for sg_ in segs)


PASSES = [Pass(0, [Seg("p", 0, 448, 0)], False), Pass(1, [Seg("p", 0, 448, 448)], False),
          Pass(2, [Seg("p", 0, 448, 896)], False), Pass(3, [Seg("p", 0, 448, 1344)], False),
          Pass(4, [Seg("p", 0, 256, 1792), Seg("s", 256, 64, 2048)], True)]

SUPERTILES = [ST(0, 0, 256, 256), ST(1, 256, 512, 512), ST(2, 768, 512, 512),
              ST(3, 1280, 512, 512), ST(4, 1792, 320, 256)]


def build_program():
    nc = bass.Bass("TRN2", target_bir_lowering=False)
    S = Sched()

    def din(name, shape, dt=F32):
        return nc.dram_tensor(name, list(shape), dt, kind="ExternalInput").ap()

    def dout(name, shape, dt=F32):
        return nc.dram_tensor(name, list(shape), dt, kind="ExternalOutput").ap()

    xT_d = din("xT", [128, NDC * NT])
    cT_d = din("cT", [128, NDC * NQ])
    sa_d = din("sa", [128, 4 * NSEQ_S * 2])
    sb_d = din("sb", [128, 4 * NSEQ_S * 30])
    vecs_d = din("vecs", [128, NV])
    ident_d = din("ident", [128, 128])
    w_ada_d = din("w_ada", [D, 6 * D])
    w_in_d = din("w_in", [D, D_IN])
    w_oa_d = din("w_out_a", [512, D])
    w_ob_d = din("w_out_b", [512, D])
    w_o_d = din("w_o", [D, D])
    wr_d = din("wr", [128, NDC * 20])
    w1_d = din("w1", [NE, D, DE])
    w3_d = din("w3", [NE, D, DE])
    w2_d = din("w2", [NE, DE, D])

    yT_d = dout("yT", [128, NDC * NT])
    nap_d = dout("nap", [128, 4 * 2])
    nbp_d = dout("nbp", [128, 4 * 30])
    nas_d = dout("nas", [128, 4 * NSEQ_S * 2])
    nbs_d = dout("nbs", [128, 4 * NSEQ_S * 30])

    NWC = 26
    wc_d = nc.dram_tensor("wcache", [NWC, 128, 2048], BF16).ap()

    w_ada_v = w_ada_d.rearrange("(kc p) n -> p kc n", p=128)
    w_in_v = w_in_d.rearrange("(kc p) n -> p kc n", p=128)
    w_oa_v = w_oa_d.rearrange("(kc p) n -> p kc n", p=128)
    w_ob_v = w_ob_d.rearrange("(kc p) n -> p kc n", p=128)
    w_o_v = w_o_d.rearrange("(kc p) n -> p kc n", p=128)
    xT_dv = xT_d.rearrange("p (c t) -> p c t", c=NDC)
    yT_dv = yT_d.rearrange("p (c t) -> p c t", c=NDC)

    es = ExitStack()
    with es:
        def sb(name, shape, dt):
            return es.enter_context(nc.sbuf_tensor(name, list(shape), dt))

        xT = sb("xT_sb", [128, NDC, NT], F32)
        vecs = sb("vecs_sb", [128, NV], F32)
        identF = sb("identF", [128, 128], F32)
        identB = sb("identB", [128, 128], BF16)
        c1024 = sb("c1024", [128, 128], BF16)
        c512 = sb("c512", [128, 128], BF16)
        modT = sb("modT", [128, 48, NQ], F32)
        gsc1 = sb("gsc1", [128, NDC, NQ], F32)
        gsc2 = sb("gsc2", [128, NDC, NQ], F32)
        cT = sb("cT_sb", [128, NDC, NQ], F32)
        scT = sb("scT", [128, NDC, NQ], BF16)
        ps_all = es.enter_context(nc.psum_tensor("ps_all", [128, 8, 512], F32))

        def PS(b, n=512):
            return ps_all[:, b, 0:n]

        def vcol(off, n=1):
            return vecs[:, off:off + n]

        def mod_scalar(which, dc, q=0):
            return modT[:, which * 8 + dc, q:q + 1]

        def bc_seq(ap3):
            return ap3.unsqueeze(2).to_broadcast([128, NSEQ_S, TS])

        def v3(ap2):
            return ap2.rearrange("p (s t) -> p s t", t=TS)


        def dma(eng, key, out_ap, in_ap, reads=(), writes=(), name=""):
            return S.add(eng, lambda e, o=out_ap, i=in_ap: [e.dma_start(out=o, in_=i)],
                         reads=reads, writes=writes, dma=key, ndma=1, name=name)

        dma(SP, "ld_vecs", vecs[:], vecs_d[:, :], writes=["vecs"], name="ld_vecs")
        dma(SP, "ld_ident", identF[:], ident_d[:, :], writes=["identF"], name="ld_ident")
        dma(SP, "ld_cT", cT[:].rearrange("p a b -> p (a b)"), cT_d[:, :], writes=["cT"], name="ld_cT")

        def xkeys(P, si):
            return [("x", P.idx, si, dc) for dc in range(NDC)]

        def load_x(P, extra_reads=()):
            for si, sg_ in enumerate(P.segs):
                dma(SP, ("ld_x", P.idx, si), xT[:, :, sg_.t0:sg_.t0 + sg_.n], xT_dv[:, :, sg_.t0:sg_.t0 + sg_.n],
                    reads=list(extra_reads), writes=xkeys(P, si), name="ld_x%d_%d" % (P.idx, si))

        load_x(PASSES[0])

        S.add(DVE, lambda e: [e.memset(c1024[:], 1.0 / 1024.0), e.memset(c512[:], 1.0 / 512.0)],
              writes=["c1024", "c512"], name="memset_consts")
        S.add(DVE, lambda e: [e.tensor_copy(identB[:], identF[:])], reads=["identF"], writes=["identB"], name="identB")
        S.add(ACT, lambda e: [e.activation(out=scT[:], in_=cT[:], func=AF.Silu)], reads=["cT"], writes=["scT"], name="silu_c")

        esA = ExitStack()
        esA.__enter__()

        def sbA(name, shape, dt):
            return esA.enter_context(nc.sbuf_tensor(name, list(shape), dt))

        NRING = 6
        ring = [sbA("ring%d" % i, [128, 2048], BF16) for i in range(NRING)]
        ring_state = {"n": 0}
        hT = sbA("hT", [128, NDC, 512], BF16)
        zsq = sbA("zsq", [128, 8, 512], BF16)
        tmp = sbA("tmp", [128, 2, 512], F32)
        rstd = sbA("rstd", [128, 512], F32)
        ba = sbA("ba", [128, 4, 512], BF16)
        ca = sbA("ca", [128, 2, 512], F32)
        sg = sbA("sg", [128, 2, 512], F32)
        ua = sbA("ua", [128, 4, 2 + 512], BF16)
        ub = sbA("ub", [128, 4, 30 + 512], BF16)
        ua_s = sbA("ua_s", [128, 4, NSEQ_S, 2 + TS], BF16)
        ub_s = sbA("ub_s", [128, 4, NSEQ_S, 30 + TS], BF16)
        vbuf = sbA("vbuf", [128, 4 * 512], F32)
        vbf = sbA("vbf", [128, 2, 512], BF16)
        vsq = sbA("vsq", [128, 2, 512], BF16)
        lnA = sbA("lnA", [128, 512], F32)
        lnB = sbA("lnB", [128, 512], F32)
        lnM = sbA("lnM", [128, 512], F32)
        sga = sbA("sga", [128, NDC, 512], BF16)
        sgb = sbA("sgb", [128, NDC, 512], BF16)
        merged = sbA("merged", [128, NDC, 512], BF16)
        mtmp = sbA("mtmp", [128, 2, 512], F32)
        dga = sbA("dga", [128, 12, 128], BF16)
        NDG = 8
        dgb = sbA("dgb", [128, NDG, 128], BF16)
        sa_t = sbA("sa_t", [128, 4, NSEQ_S, 2], F32)
        nap_t = sbA("nap_t", [128, 4, 2], F32)
        nbp_t = sbA("nbp_t", [128, 4, 30], F32)
        nas_t = sbA("nas_t", [128, 4, NSEQ_S, 2], F32)
        nbs_t = sbA("nbs_t", [128, 4, NSEQ_S, 30], F32)

        def V(c, n=512):
            return vbuf[:, c * 512:c * 512 + n]

        sb_stage = vbuf[:, 0:4 * NSEQ_S * 30].rearrange("p (c s j) -> p c s j", c=4, s=NSEQ_S)
        dma(SP, "ld_sa", sa_t[:].rearrange("p a b c -> p (a b c)"), sa_d[:, :], writes=["sa_t"], name="ld_sa")
        dma(SP, "ld_sb", vbuf[:, 0:4 * NSEQ_S * 30], sb_d[:, :], writes=[("v", c) for c in range(4)], name="ld_sb")
        S.add(DVE, lambda e: [e.tensor_copy(ua_s[:, :, :, 0:2], sa_t[:])], reads=["sa_t"], writes=["ua_s"], name="ua_s_hist")
        S.add(DVE, lambda e: [e.tensor_copy(ub_s[:, :, :, 0:30], sb_stage)],
              reads=[("v", c) for c in range(4)], writes=["ub_s"], name="ub_s_hist")
        S.add(ACT, lambda e: [e.activation(out=nbs_t[:, :, :, 0:26], in_=sb_stage[:, :, :, 4:30], func=AF.Copy)],
              reads=[("v", c) for c in range(4)], writes=["nbs_t"], name="nbs_hist")
        S.add(DVE, lambda e: [e.memset(ua[:, :, 0:2], 0.0), e.memset(ub[:, :, 0:30], 0.0)],
              writes=["ua_halo", "ub_halo"], name="halo0")
        def mk_dga(e):
            out = []
            for c in range(4):
                for k in range(3):
                    out.append(e.tensor_scalar(dga[:, c * 3 + k, :], identB[:], vcol(V_CAW + c * 3 + k), None, ALU.mult))
            return out
        S.add(DVE, mk_dga, reads=["identB", "vecs"], writes=["dga"], name="mk_dga")

        slot_pending = {}

        def ring_load(src_ap, shape3, name, store_as=None):
            slot = ring_state["n"] % NRING
            ring_state["n"] += 1
            a_, b_ = shape3
            assert a_ * b_ == 2048
            for ps_, (seq_, bid) in list(slot_pending.items()):
                if ring_state["n"] - seq_ >= 3 or ps_ == slot:
                    slot_pending.pop(ps_)
                    S.add(POOL, lambda e, bid=bid, ps_=ps_: [e.dma_start(out=wc_d[bid], in_=ring[ps_][:, :])],
                          reads=[("ring", ps_)], writes=[("wc", bid)], dma=("wcst", ps_), ndma=1, name="wc_store")
            view = ring[slot][:, 0:a_ * b_].rearrange("p (a b) -> p a b", a=a_)
            if src_ap is None:
                S.add(POOL, lambda e, slot=slot, bid=store_as: [e.dma_start(out=ring[slot][:, :], in_=wc_d[bid])],
                      reads=[("wc", store_as)], writes=[("ring", slot)], dma=("ring", slot), ndma=1, name=name)
            else:
                S.add(POOL, lambda e, o=view, i=src_ap: [e.dma_start(out=o, in_=i)],
                      writes=[("ring", slot)], dma=("ring", slot), ndma=1, name=name)
                if store_as is not None:
                    slot_pending[slot] = (ring_state["n"], store_as)
            return slot, view

        wcache = {}
        WC_BASE = {"w_in": 0, "w_o": 18, "w_oa": 22, "w_ob": 24}

        def wblk(kind, tag, idx):
            key = (kind, tag, idx)
            if key not in wcache:
                if kind == "w_ada":
                    wcache[key] = ring_load(w_ada_v[:, :, idx * 256:(idx + 1) * 256], (NDC, 256), "w_ada")
                else:
                    bid = WC_BASE[kind] + idx
                    shape = (4, 512) if kind in ("w_oa", "w_ob") else (NDC, 256)
                    if tag == 0:
                        if kind == "w_in":
                            src = w_in_v[:, :, idx * 256:(idx + 1) * 256]
                        elif kind == "w_o":
                            src = w_o_v[:, :, idx * 256:(idx + 1) * 256]
                        elif kind == "w_oa":
                            src = w_oa_v[:, :, idx * 512:(idx + 1) * 512]
                        else:
                            src = w_ob_v[:, :, idx * 512:(idx + 1) * 512]
                        wcache[key] = ring_load(src, shape, kind, store_as=bid)
                    else:
                        wcache[key] = ring_load(None, shape, kind + "_c", store_as=bid)
            return wcache[key]

        misc_bank = {"n": 0}

        def next_misc_bank():
            b = 2 + (misc_bank["n"] % 4)
            misc_bank["n"] += 1
            return b


        def mod_load(hb):
            wblk("w_ada", 0, hb)

        def mod_compute(hb):
            slot, wv = wblk("w_ada", 0, hb)
            bank = next_misc_bank()

            def mm(e):
                out = []
                for jj in range(2):
                    for kc in range(NDC):
                        out.append(e.matmul(ps_all[:, bank, jj * NQ:(jj + 1) * NQ], wv[:, kc, jj * 128:(jj + 1) * 128],
                                            scT[:, kc, :], start=(kc == 0), stop=(kc == NDC - 1)))
                return out
            S.add(PE, mm, reads=[("ring", slot), "scT"], writes=[("ps", bank)], name="mod_mm%d" % hb)

            def ev(e):
                out = []
                for jj in range(2):
                    j = hb * 2 + jj
                    out.append(e.activation(out=modT[:, j, :], in_=ps_all[:, bank, jj * NQ:(jj + 1) * NQ],
                                            func=AF.Identity, bias=vcol(V_BADA + j), scale=1.0))
                return out
            S.add(ACT, ev, reads=[("ps", bank), "vecs"], writes=[("modT", hb)], name="mod_ev%d" % hb)

        def mod_block(hb):
            mod_load(hb)
            mod_compute(hb)

        def mod_keys(which):
            return [("modT", which * 4 + i) for i in range(4)]

        def mk_gsc(which_sc, gvec_off, gsc, name):
            def f(e):
                i1 = e.tensor_scalar(gsc[:], modT[:, which_sc * 8:(which_sc + 1) * 8, :], 1.0, None, ALU.add)
                i2 = e.tensor_tensor(gsc[:], gsc[:], vecs[:, gvec_off:gvec_off + 8].unsqueeze(2).to_broadcast([128, NDC, NQ]), ALU.mult)
                return [i1, i2]
            S.add(DVE, lambda e: [e.tensor_scalar(gsc[:], modT[:, which_sc * 8:(which_sc + 1) * 8, :], 1.0, None, ALU.add)],
                  reads=mod_keys(which_sc), writes=[name + "_t"], name=name + "_a")
            S.add(DVE, lambda e: [e.tensor_tensor(gsc[:], gsc[:], vecs[:, gvec_off:gvec_off + 8].unsqueeze(2).to_broadcast([128, NDC, NQ]), ALU.mult)],
                  reads=[name + "_t", "vecs"], writes=[name], name=name + "_b")

        def CS(sg_):
            return slice(sg_.c0, sg_.c0 + sg_.n)

        def norm1_sq(P):
            sqk = [("zsq", i) for i in range(8)]
            for si, sg_ in enumerate(P.segs):
                S.add(ACT, lambda e, sg_=sg_: [e.activation(out=zsq[:, :, CS(sg_)], in_=xT[:, :, sg_.t0:sg_.t0 + sg_.n], func=AF.Square)],
                      reads=xkeys(P, si), writes=sqk, name="n1_sq%d" % P.idx)

        def norm1(P):
            n = P.n
            sqk = [("zsq", i) for i in range(8)]
            bank = next_misc_bank()
            S.add(PE, lambda e: [e.matmul(PS(bank, n), c1024[:], zsq[:, dc, 0:n], start=(dc == 0), stop=(dc == NDC - 1)) for dc in range(NDC)],
                  reads=sqk + ["c1024"], writes=[("ps", bank)], name="n1_ss%d" % P.idx)
            S.add(ACT, lambda e: [e.activation(out=rstd[:, 0:n], in_=PS(bank, n), func=AF.Ln, bias=EPS, scale=1.0)],
                  reads=[("ps", bank)], writes=["rstd"], name="n1_ln%d" % P.idx)
            S.add(ACT, lambda e: [e.activation(out=rstd[:, 0:n], in_=rstd[:, 0:n], func=AF.Exp, scale=-0.5)], reads=["rstd"], writes=["rstd"],
                  name="n1_exp%d" % P.idx)
        def norm1_dc(P, dc):
            if True:
                r = dc % 2
                for si, sg_ in enumerate(P.segs):
                    cs = CS(sg_)
                    xs = xT[:, dc, sg_.t0:sg_.t0 + sg_.n]
                    if not sg_.sample:
                        S.add(DVE, lambda e, xs=xs, dc=dc, r=r, cs=cs: [e.scalar_tensor_tensor(
                            tmp[:, r, cs], xs, gsc1[:, dc, 0:1], rstd[:, cs], ALU.mult, ALU.mult)],
                            reads=[("x", P.idx, si, dc), "gsc1", "rstd"], writes=[("tmp", r)], name="n1_t")
                        S.add(ACT, lambda e, dc=dc, r=r, cs=cs: [e.activation(
                            out=hT[:, dc, cs], in_=tmp[:, r, cs], func=AF.Identity, bias=mod_scalar(0, dc), scale=1.0)],
                            reads=[("tmp", r)] + mod_keys(0), writes=[("hT", dc)], name="n1_h")
                    else:
                        S.add(DVE, lambda e, xs=xs, r=r, cs=cs: [e.tensor_tensor(tmp[:, r, cs], xs, rstd[:, cs], ALU.mult)],
                              reads=[("x", P.idx, si, dc), "rstd"], writes=[("tmp", r)], name="n1s_a")
                        S.add(DVE, lambda e, dc=dc, r=r, cs=cs: [e.tensor_tensor(
                            v3(tmp[:, r, cs]), v3(tmp[:, r, cs]), bc_seq(gsc1[:, dc, 1:NQ]), ALU.mult)],
                            reads=[("tmp", r), "gsc1"], writes=[("tmp", r)], name="n1s_b")
                        S.add(DVE, lambda e, dc=dc, r=r, cs=cs: [e.tensor_tensor(
                            v3(hT[:, dc, cs]), v3(tmp[:, r, cs]), bc_seq(modT[:, 0 * 8 + dc, 1:NQ]), ALU.add)],
                            reads=[("tmp", r)] + mod_keys(0), writes=[("hT", dc)], name="n1s_c")

        proj_bank = {"n": 0}

        def next_proj_bank():
            b = proj_bank["n"] % 2
            proj_bank["n"] += 1
            return b

        def proj_chunk(P, m):
            n = P.n
            slot, wv = wblk("w_in", P.idx, m // 2)
            j = m % 2
            bank = next_proj_bank()

            def mm(e):
                return [e.matmul(PS(bank, n), wv[:, kc, j * 128:(j + 1) * 128], hT[:, kc, 0:n],
                                 start=(kc == 0), stop=(kc == NDC - 1)) for kc in range(NDC)]
            S.add(PE, mm, reads=[("ring", slot)] + [("hT", dc) for dc in range(NDC)], writes=[("ps", bank)], name="proj_mm")
            return bank

        def in_proj(P, filler=()):
            n = P.n
            filler = list(filler)

            def fill(k):
                for _ in range(k):
                    if filler:
                        filler.pop(0)()
            for c in range(4):
                r = c % 2
                bk = proj_chunk(P, 4 + c)
                S.add(ACT, lambda e, bk=bk, r=r: [e.activation(out=ca[:, r, 0:n], in_=PS(bk, n), func=AF.Copy)],
                      reads=[("ps", bk)], writes=[("ca", r)], name="ca_ev")
                bk2 = proj_chunk(P, 8 + c)
                for sg_ in P.segs:
                    cs = CS(sg_)
                    if not sg_.sample:
                        np_ = sg_.n
                        S.add(DVE, lambda e, bk2=bk2, r=r, c=c, cs=cs, np_=np_: [e.tensor_tensor(ua[:, c, 2:2 + np_], PS(bk2, n)[:, cs], ca[:, r, cs], ALU.mult)],
                              reads=[("ps", bk2), ("ca", r)], writes=[("ua", c)], name="ua_ev")
                        if P.last_prompt:
                            S.add(DVE, lambda e, bk2=bk2, r=r, c=c, np_=np_: [e.tensor_tensor(nap_t[:, c, :], PS(bk2, n)[:, np_ - 2:np_], ca[:, r, np_ - 2:np_], ALU.mult)],
                                  reads=[("ps", bk2), ("ca", r)], writes=["nap_t"], name="nap_ev")
                    else:
                        S.add(DVE, lambda e, bk2=bk2, r=r, c=c, cs=cs: [e.tensor_tensor(ua_s[:, c, :, 2:2 + TS], v3(PS(bk2, n)[:, cs]), v3(ca[:, r, cs]), ALU.mult)],
                              reads=[("ps", bk2), ("ca", r), "ua_s"], writes=[("uas", c)], name="uas_ev")
                        S.add(DVE, lambda e, bk2=bk2, r=r, c=c, cs=cs: [e.tensor_tensor(nas_t[:, c, :, :], v3(PS(bk2, n)[:, cs])[:, :, 2:4], v3(ca[:, r, cs])[:, :, 2:4], ALU.mult)],
                              reads=[("ps", bk2), ("ca", r)], writes=["nas_t"], name="nas_ev")
                fill(2)
            for c in range(4):
                r = c % 2
                bk = proj_chunk(P, 16 + c)
                S.add(ACT, lambda e, bk=bk, r=r: [e.activation(out=sg[:, r, 0:n], in_=PS(bk, n), func=AF.Sigmoid)],
                      reads=[("ps", bk)], writes=[("sg", r)], name="sg_ev")
                bk2 = proj_chunk(P, 12 + c)
                for sg_ in P.segs:
                    cs = CS(sg_)
                    if not sg_.sample:
                        np_ = sg_.n
                        S.add(DVE, lambda e, bk2=bk2, r=r, c=c, cs=cs, np_=np_: [e.tensor_tensor(ub[:, c, 30:30 + np_], PS(bk2, n)[:, cs], sg[:, r, cs], ALU.mult)],
                              reads=[("ps", bk2), ("sg", r)], writes=[("ub", c)], name="ub_ev")
                        if P.last_prompt:
                            S.add(DVE, lambda e, bk2=bk2, r=r, c=c, np_=np_: [e.tensor_tensor(nbp_t[:, c, :], PS(bk2, n)[:, np_ - 30:np_], sg[:, r, np_ - 30:np_], ALU.mult)],
                                  reads=[("ps", bk2), ("sg", r)], writes=["nbp_t"], name="nbp_ev")
                    else:
                        S.add(DVE, lambda e, bk2=bk2, r=r, c=c, cs=cs: [e.tensor_tensor(ub_s[:, c, :, 30:30 + TS], v3(PS(bk2, n)[:, cs]), v3(sg[:, r, cs]), ALU.mult)],
                              reads=[("ps", bk2), ("sg", r), "ub_s"], writes=[("ubs", c)], name="ubs_ev")
                        S.add(DVE, lambda e, bk2=bk2, r=r, c=c, cs=cs: [e.tensor_tensor(nbs_t[:, c, :, 26:30], v3(PS(bk2, n)[:, cs]), v3(sg[:, r, cs]), ALU.mult)],
                              reads=[("ps", bk2), ("sg", r), "nbs_t"], writes=["nbs_t2"], name="nbs_ev")
                fill(3)
            fill(100)
        def in_proj_ba(P):
            n = P.n
            for c in range(4):
                bk = proj_chunk(P, c)
                S.add(ACT, lambda e, bk=bk, c=c: [e.activation(out=ba[:, c, 0:n], in_=PS(bk, n), func=AF.Copy)],
                      reads=[("ps", bk)], writes=[("ba", c)], name="ba_ev")

        def in_proj_gates(P, filler=()):
            n = P.n
            filler = list(filler)
            for half, dst, nm in ((0, sga, "sga"), (1, sgb, "sgb")):
                for m in range(8):
                    bk = proj_chunk(P, 20 + half * 8 + m)
                    S.add(ACT, lambda e, bk=bk, m=m, dst=dst: [e.activation(out=dst[:, m, 0:n], in_=PS(bk, n), func=AF.Sigmoid)],
                          reads=[("ps", bk)], writes=[(nm, m)], name=nm + "_ev")
                    for _ in range(2):
                        if filler:
                            filler.pop(0)()
            while filler:
                filler.pop(0)()

        dg_state = {}

        def gen_diags(P, upto):
            nxt_ = dg_state.get(P.idx, 0)
            upto = min(upto, 4 * 31 - 1)
            while nxt_ <= upto:
                c, k = divmod(nxt_, 31)
                r = nxt_ % NDG
                if nxt_ % 6 == 1:
                    S.add(ACT, lambda e, c=c, k=k, r=r: [e.activation(out=dgb[:, r, :], in_=identB[:], func=AF.Copy, scale=vcol(V_CBW + c * 31 + k))],
                          reads=["identB", "vecs"], writes=[("dgb", r)], name="mk_dgb_act")
                else:
                    S.add(DVE, lambda e, c=c, k=k, r=r: [e.tensor_scalar(dgb[:, r, :], identB[:], vcol(V_CBW + c * 31 + k), None, ALU.mult)],
                          reads=["identB", "vecs"], writes=[("dgb", r)], name="mk_dgb")
                nxt_ += 1
            dg_state[P.idx] = nxt_

        def diag_slot(P, j):
            gen_diags(P, j)
            return j % NDG

        def convs(P, filler=()):
            n = P.n
            filler = list(filler)

            def mv_a(sg_, c, k):
                return ua_s[:, c, :, k:k + TS] if sg_.sample else ua[:, c, k:k + sg_.n]

            def mv_b(sg_, c, k):
                return ub_s[:, c, :, k:k + TS] if sg_.sample else ub[:, c, k:k + sg_.n]

            def outp(sg_, bank):
                return v3(PS(bank, sg_.n)) if sg_.sample else PS(bank, sg_.n)

            for c in range(4):
                banks = [next_misc_bank() for _ in P.segs]

                def mm(e, c=c, banks=banks):
                    out = []
                    for k in range(3):
                        for sg_, bank in zip(P.segs, banks):
                            out.append(e.matmul(outp(sg_, bank), dga[:, c * 3 + k, :], mv_a(sg_, c, k), start=(k == 0), stop=(k == 2)))
                    return out
                S.add(PE, mm, reads=["dga", ("ua", c), ("uas", c), "ua_halo", "ua_s"], writes=[("ps", bk_) for bk_ in banks], name="conva_mm")
                for sg_, bank in zip(P.segs, banks):
                    S.add(DVE, lambda e, c=c, bank=bank, sg_=sg_: [e.tensor_tensor(zsq[:, c, CS(sg_)], PS(bank, sg_.n), ba[:, c, CS(sg_)], ALU.mult)],
                          reads=[("ps", bank), ("ba", c)], writes=[("zsq", c)], name="za_ev")
            if filler:
                filler.pop(0)()
            def stats(c):
                r2 = c % 2
                S.add(PE, lambda e, c=c, r2=r2: [e.matmul(PS(6, n), c512[:], vbf[:, r2, 0:n], start=(c == 0), stop=(c == 3))],
                      reads=[("vbf", r2), "c512"], writes=[("ps", 6)] if c == 0 else [("psMacc", c)], name="ln_mean_mm")
                S.add(PE, lambda e, c=c, r2=r2: [e.matmul(PS(7, n), c512[:], vsq[:, r2, 0:n], start=(c == 0), stop=(c == 3))],
                      reads=[("vsq", r2), "c512"], writes=[("ps", 7)] if c == 0 else [("psEacc", c)], name="ln_ex2_mm")

            for c in range(4):
                banks = [next_misc_bank() for _ in P.segs]
                for k in range(31):
                    r = diag_slot(P, c * 31 + k)

                    def mm(e, c=c, k=k, r=r, banks=banks):
                        return [e.matmul(outp(sg_, bank), dgb[:, r, :], mv_b(sg_, c, k), start=(k == 0), stop=(k == 30))
                                for sg_, bank in zip(P.segs, banks)]
                    S.add(PE, mm, reads=[("dgb", r), ("ub", c), ("ubs", c), "ub_halo", "ub_s"],
                          writes=[("ps", bk_) for bk_ in banks] if k == 0 else [("psCacc", bk_) for bk_ in banks], name="convb_mm")
                    gen_diags(P, c * 31 + k + NDG)
                    if filler and (c * 31 + k) % 12 == 11:
                        filler.pop(0)()
                r2 = c % 2
                for sg_, bank in zip(P.segs, banks):
                    cs = CS(sg_)
                    rk = [("ps", bank), ("psCacc", bank), "vecs"]
                    S.add(ACT, lambda e, c=c, bank=bank, sg_=sg_, cs=cs: [e.activation(out=V(c, n)[:, cs], in_=PS(bank, sg_.n), func=AF.Identity, bias=vcol(V_CBB + c), scale=1.0)],
                          reads=rk, writes=[("v", c)], name="v_ev")
                    S.add(ACT, lambda e, c=c, bank=bank, r2=r2, sg_=sg_, cs=cs: [e.activation(out=vbf[:, r2, cs], in_=PS(bank, sg_.n), func=AF.Identity, bias=vcol(V_CBB + c), scale=1.0)],
                          reads=rk, writes=[("vbf", r2)], name="vbf_ev")
                    S.add(ACT, lambda e, c=c, bank=bank, r2=r2, sg_=sg_, cs=cs: [e.activation(out=vsq[:, r2, cs], in_=PS(bank, sg_.n), func=AF.Square, bias=vcol(V_CBB + c), scale=1.0)],
                          reads=rk, writes=[("vsq", r2)], name="vsq_ev")
                if c >= 1:
                    stats(c - 1)
            stats(3)
            while filler:
                filler.pop(0)()
            if not P.last_prompt:
                np_ = P.segs[0].n
                S.add(POOL, lambda e: [e.tensor_copy(ua[:, :, 0:2], ua[:, :, np_:np_ + 2])],
                      reads=[("ua", c) for c in range(4)], writes=["ua_halo"], name="ua_halo")
                S.add(POOL, lambda e: [e.tensor_copy(ub[:, :, 0:30], ub[:, :, np_:np_ + 30])],
                      reads=[("ub", c) for c in range(4)], writes=["ub_halo"], name="ub_halo")

        def layernorm_b(P):
            n = P.n
            ops = []
            eb = 7
            accs = [("psMacc", c) for c in range(1, 4)]
            accE = [("psEacc", c) for c in range(1, 4)]
            ops.append(lambda: S.add(ACT, lambda e: [e.activation(out=lnM[:, 0:n], in_=PS(6, n), func=AF.Copy)],
                  reads=[("ps", 6)] + accs, writes=["lnM"], name="ln_m"))
            ops.append(lambda: S.add(DVE, lambda e: [e.tensor_tensor(lnB[:, 0:n], lnM[:, 0:n], lnM[:, 0:n], ALU.mult)],
                  reads=["lnM"], writes=["lnB"], name="ln_m2"))
            ops.append(lambda: S.add(DVE, lambda e: [e.tensor_tensor(lnA[:, 0:n], PS(eb, n), lnB[:, 0:n], ALU.subtract)],
                  reads=[("ps", eb), "lnB"] + accE, writes=["lnA"], name="ln_var"))
            ops.append(lambda: S.add(ACT, lambda e: [e.activation(out=lnA[:, 0:n], in_=lnA[:, 0:n], func=AF.Ln, bias=EPS, scale=1.0)],
                  reads=["lnA"], writes=["lnA"], name="ln_ln"))
            ops.append(lambda: S.add(ACT, lambda e: [e.activation(out=lnA[:, 0:n], in_=lnA[:, 0:n], func=AF.Exp, scale=-0.5)],
                  reads=["lnA"], writes=["lnA"], name="ln_exp"))
            ops.append(lambda: S.add(DVE, lambda e: [e.scalar_tensor_tensor(lnB[:, 0:n], lnM[:, 0:n], -1.0, lnA[:, 0:n], ALU.mult, ALU.mult)],
                  reads=["lnM", "lnA"], writes=["lnB"], name="ln_B"))
            for c in range(4):
                ops.append(lambda c=c: S.add(DVE, lambda e, c=c: [e.tensor_tensor(V(c, n), V(c, n), lnA[:, 0:n], ALU.mult)],
                      reads=[("v", c), "lnA"], writes=[("v", c)], name="ln_a"))
                ops.append(lambda c=c: S.add(DVE, lambda e, c=c: [e.tensor_tensor(V(c, n), V(c, n), lnB[:, 0:n], ALU.add)],
                      reads=[("v", c), "lnB"], writes=[("v", c)], name="ln_b"))
                ops.append(lambda c=c: S.add(ACT, lambda e, c=c: [e.activation(out=zsq[:, 4 + c, 0:n], in_=V(c, n), func=AF.Silu,
                                                        bias=vcol(V_LNB + c), scale=vcol(V_LNG + c))],
                      reads=[("v", c), "vecs"], writes=[("zsq", 4 + c)], name="zb_ev"))
            return ops

        def mixer_out(P):
            n = P.n
            for m in range(NDC):
                sa_, wav = wblk("w_oa", P.idx, m // 4)
                sb_, wbv = wblk("w_ob", P.idx, m // 4)
                j = m % 4
                bka = next_misc_bank()
                bkb = next_misc_bank()
                S.add(PE, lambda e, j=j, bka=bka, wav=wav: [e.matmul(PS(bka, n), wav[:, kc, j * 128:(j + 1) * 128], zsq[:, kc, 0:n],
                                                                      start=(kc == 0), stop=(kc == 3)) for kc in range(4)],
                      reads=[("ring", sa_)] + [("zsq", c) for c in range(4)], writes=[("ps", bka)], name="ya_mm")
                S.add(PE, lambda e, j=j, bkb=bkb, wbv=wbv: [e.matmul(PS(bkb, n), wbv[:, kc, j * 128:(j + 1) * 128], zsq[:, 4 + kc, 0:n],
                                                                      start=(kc == 0), stop=(kc == 3)) for kc in range(4)],
                      reads=[("ring", sb_)] + [("zsq", 4 + c) for c in range(4)], writes=[("ps", bkb)], name="yb_mm")
                S.add(DVE, lambda e, m=m, bka=bka: [e.tensor_tensor(mtmp[:, 0, 0:n], PS(bka, n), sga[:, m, 0:n], ALU.mult)],
                      reads=[("ps", bka), ("sga", m)], writes=[("mtmp", 0)], name="mg_a")
                S.add(DVE, lambda e, m=m, bkb=bkb: [e.tensor_tensor(mtmp[:, 1, 0:n], PS(bkb, n), sgb[:, m, 0:n], ALU.mult)],
                      reads=[("ps", bkb), ("sgb", m)], writes=[("mtmp", 1)], name="mg_b")
                S.add(DVE, lambda e, m=m: [e.tensor_tensor(merged[:, m, 0:n], mtmp[:, 0, 0:n], mtmp[:, 1, 0:n], ALU.add)],
                      reads=[("mtmp", 0), ("mtmp", 1)], writes=[("merged", m)], name="mg_sum")

        def out_proj(P):
            n = P.n
            for m in range(NDC):
                so_, wov = wblk("w_o", P.idx, m // 2)
                j = m % 2
                bk = next_misc_bank()
                S.add(PE, lambda e, j=j, bk=bk, wov=wov: [e.matmul(PS(bk, n), wov[:, kc, j * 128:(j + 1) * 128], merged[:, kc, 0:n],
                                                                    start=(kc == 0), stop=(kc == NDC - 1)) for kc in range(NDC)],
                      reads=[("ring", so_)] + [("merged", c) for c in range(NDC)], writes=[("ps", bk)], name="wo_mm")
                for si, sg_ in enumerate(P.segs):
                    cs = CS(sg_)
                    xs = xT[:, m, sg_.t0:sg_.t0 + sg_.n]
                    if not sg_.sample:
                        S.add(DVE, lambda e, m=m, bk=bk, xs=xs, cs=cs: [e.scalar_tensor_tensor(xs, PS(bk, n)[:, cs], mod_scalar(2, m), xs, ALU.mult, ALU.add)],
                              reads=[("ps", bk), ("x", P.idx, si, m)] + mod_keys(2), writes=[("x", P.idx, si, m)], name="x2_ev")
                    else:
                        S.add(DVE, lambda e, m=m, bk=bk, cs=cs: [e.tensor_tensor(v3(mtmp[:, 0, cs]), v3(PS(bk, n)[:, cs]), bc_seq(modT[:, 2 * 8 + m, 1:NQ]), ALU.mult)],
                              reads=[("ps", bk)] + mod_keys(2), writes=[("mtmp", 0)], name="x2s_a")
                        S.add(DVE, lambda e, xs=xs, cs=cs: [e.tensor_tensor(xs, xs, mtmp[:, 0, cs], ALU.add)],
                              reads=[("mtmp", 0), ("x", P.idx, si, m)], writes=[("x", P.idx, si, m)], name="x2s_b")

        norm1_sq(PASSES[0])
        norm1(PASSES[0])
        for hb in range(8):
            mod_block(hb)
        mk_gsc(1, V_N1G, gsc1, "gsc1")
        for dc in range(NDC):
            norm1_dc(PASSES[0], dc)
        in_proj(PASSES[0])
        in_proj_ba(PASSES[0])
        mod_sched = {0: [8, 9, 10, 11], 2: [16, 17, 18, 19], 3: [20, 21, 22, 23], 4: [12, 13, 14, 15]}
        for P in PASSES:
            nxt = PASSES[P.idx + 1] if P.idx + 1 < len(PASSES) else None
            if nxt is not None:
                load_x(nxt, extra_reads=[("hT", 0)])
                norm1_sq(nxt)
            if nxt is not None:
                in_proj_gates(P)
            gen_diags(P, NDG - 1)
            if nxt is not None:
                norm1(nxt)
            mods = mod_sched.get(P.idx, [])
            for hb in mods[:4]:
                mod_load(hb)
            if nxt is not None:
                wblk("w_in", nxt.idx, 2)
                wblk("w_in", nxt.idx, 4)
            convs(P, filler=[(lambda dc=dc: norm1_dc(nxt, dc)) for dc in range(NDC)] if nxt is not None else ())
            ln_ops = layernorm_b(P)
            if nxt is None:
                for hb in mods:
                    mod_compute(hb)
                mods = []
                in_proj_gates(P, filler=ln_ops)
                ln_ops = []
            for hb in mods:
                mod_compute(hb)
            if nxt is not None:
                in_proj(nxt, filler=ln_ops)
            mixer_out(P)
            if nxt is not None:
                in_proj_ba(nxt)
            if P.idx == 4:
                mk_gsc(4, V_N2G, gsc2, "gsc2")
            out_proj(P)

        def out_dma(key, dst, src, reads, name):
            return S.add(SP, lambda e: [e.dma_start(out=dst, in_=src)], reads=reads, writes=[("out", key)], dma=("out", key), ndma=1, name=name)
        out_dma("nap", nap_d[:, :], nap_t[:].rearrange("p a b -> p (a b)"), ["nap_t"], "st_nap")
        out_dma("nbp", nbp_d[:, :], nbp_t[:].rearrange("p a b -> p (a b)"), ["nbp_t"], "st_nbp")
        out_dma("nas", nas_d[:, :], nas_t[:].rearrange("p a b c -> p (a b c)"), ["nas_t"], "st_nas")
        out_dma("nbs", nbs_d[:, :], nbs_t[:].rearrange("p a b c -> p (a b c)"), ["nbs_t", "nbs_t2"], "st_nbs")

        all_keys = list(S.last_w.keys())
        S.add(SP, lambda e: [], writes=all_keys + ["__bar__"], name="barrier_sp")
        for eng in (PE, ACT, DVE, POOL):
            S.add(eng, lambda e: [], reads=["__bar__"], name="fence_" + eng)
        esA.__exit__(None, None, None)

        def sbB(name, shape, dt):
            return es.enter_context(nc.sbuf_tensor(name, list(shape), dt))

        h2T = sbB("h2T", [128, NDC, NT], BF16)
        h2f = sbB("h2f", [128, NDC, 512], F32)
        sqB = sbB("sqB", [128, NDC, 512], BF16)
        rstdB = sbB("rstdB", [128, 512], F32)
        wr_sb = sbB("wr_sb", [128, NDC, 20], F32)
        LT = sbB("LT", [20, 512], F32)
        NTT = 17
        L = sbB("L", [128, NTT, 20], F32)
        gmax = sbB("gmax", [128, NTT], F32)
        gmask = sbB("gmask", [128, NTT, 4], F32)
        gsh = sbB("gsh", [128, NTT, 4], F32)
        gsum = sbB("gsum", [128, NTT], F32)
        pg = sbB("pg", [128, NTT], F32)
        pen = sbB("pen", [128, NTT, 4], F32)
        Em = sbB("Em", [128, NTT, 16], F32)
        Em2 = Em
        m1 = sbB("m1", [128, NTT, 16], F32)
        m2 = sbB("m2", [128, NTT, 16], F32)
        v1 = sbB("v1", [128, NTT], F32)
        v2 = sbB("v2", [128, NTT], F32)
        dd = sbB("dd", [128, NTT], F32)
        w1s = sbB("w1s", [128, NTT], F32)
        w2s = sbB("w2s", [128, NTT], F32)
        comb = m1
        combT = sbB("combT", [16, NT], BF16)
        sel = sbB("sel", [16, 16, 128], BF16)
        NWS = 2
        wA = [sbB("wA%d" % i, [128, 2, NDC, DE], BF16) for i in range(NWS)]
        wB = [sbB("wB%d" % i, [128, 2, NDC, DE], BF16) for i in range(NWS)]
        wC = [sbB("wC%d" % i, [128, 2, 2, D], BF16) for i in range(NWS)]
        abuf = [sqB[:, 0:4, :], sqB[:, 4:8, :]]
        slb = sbB("slb", [128, 2, 512], F32)
        tmpB = slb
        tlb = sbB("tlb", [128, 2, 512], F32)
        gtmp = sbB("gtmp", [128, 512], F32)

        FK = []

        dma(SP, "ld_wr", wr_sb[:].rearrange("p a b -> p (a b)"), wr_d[:, :], reads=FK, writes=["wr_sb"], name="ld_wr")

        def load_pair(p):
            slot = p % NWS
            for el in range(2):
                e_ = 2 * p + el
                S.add(POOL, lambda e, e_=e_, el=el, slot=slot: [
                    e.dma_start(out=wA[slot][:, el, :, :], in_=w1_d[e_].rearrange("(kc p) f -> p kc f", p=128)),
                    e.dma_start(out=wB[slot][:, el, :, :], in_=w3_d[e_].rearrange("(kc p) f -> p kc f", p=128)),
                    e.dma_start(out=wC[slot][:, el, :, :], in_=w2_d[e_].rearrange("(fc p) d -> p fc d", p=128))],
                    reads=FK, writes=[("wexp", slot, el)], dma=("wexp", slot, el), ndma=3, name="ld_pair%d_%d" % (p, el))

        load_pair(0)
        load_pair(1)

        S.add(DVE, lambda e: [e.memset(L[:], 0.0)], reads=FK, writes=["L"], name="L0")
        S.add(DVE, lambda e: [e.tensor_copy(sel[:], identF[0:16, 0:16].unsqueeze(2).to_broadcast([16, 16, 128]))],
              reads=FK + ["identF"], writes=["sel"], name="mk_sel")

        rbufs = [rstdB, gtmp]
        rkeys = ["rstdB", "gtmp"]

        def n2_A(st):
            n = st.n
            rb, rk = rbufs[st.idx % 2], rkeys[st.idx % 2]
            sqk = [("h2T", st.idx, i) for i in range(8)]
            S.add(ACT, lambda e: [e.activation(out=h2T[:, :, st.t0:st.t0 + n], in_=xT[:, :, st.t0:st.t0 + n], func=AF.Square)],
                  reads=[("x", st.idx, dc) for dc in range(NDC)], writes=sqk, name="n2_sq")

        def n2_A2(st):
            n = st.n
            rb, rk = rbufs[st.idx % 2], rkeys[st.idx % 2]
            sqk = [("h2T", st.idx, i) for i in range(8)]
            sbank = st.idx % 2
            S.add(PE, lambda e: [e.matmul(PS(sbank, n), c1024[:], h2T[:, dc, st.t0:st.t0 + n], start=(dc == 0), stop=(dc == NDC - 1)) for dc in range(NDC)],
                  reads=sqk + ["c1024"], writes=[("ps", sbank)], name="n2_ss")
            S.add(ACT, lambda e: [e.activation(out=rb[:, 0:n], in_=PS(sbank, n), func=AF.Ln, bias=EPS, scale=1.0)],
                  reads=[("ps", sbank)], writes=[rk], name="n2_ln")
            S.add(ACT, lambda e: [e.activation(out=rb[:, 0:n], in_=rb[:, 0:n], func=AF.Exp, scale=-0.5)], reads=[rk], writes=[rk], name="n2_exp")

        def n2_B(st):
            n = st.n
            npr = st.npr
            rb, rk = rbufs[st.idx % 2], rkeys[st.idx % 2]
            for dc in range(NDC):
                xs = xT[:, dc, st.t0:st.t0 + npr]
                S.add(DVE, lambda e, xs=xs, dc=dc: [e.scalar_tensor_tensor(
                    h2f[:, dc, 0:npr], xs, gsc2[:, dc, 0:1], rb[:, 0:npr], ALU.mult, ALU.mult)],
                    reads=[("x", st.idx, dc), "gsc2", rk], writes=[("h2f", dc)], name="n2_t")
            for dc in range(NDC):
                S.add(ACT, lambda e, dc=dc: [e.activation(
                    out=h2f[:, dc, 0:npr], in_=h2f[:, dc, 0:npr], func=AF.Identity, bias=mod_scalar(3, dc), scale=1.0)],
                    reads=[("h2f", dc)] + mod_keys(3), writes=[("h2f", dc)], name="n2_h")
            if npr < n:
                for dc in range(NDC):
                    r = dc % 2
                    xs = xT[:, dc, st.t0 + npr:st.t0 + n]
                    S.add(DVE, lambda e, xs=xs, r=r: [e.tensor_tensor(tmpB[:, r, npr:n], xs, rb[:, npr:n], ALU.mult)],
                          reads=[("x", st.idx, dc), rk], writes=[("slb", r)], name="n2s_a")
                    S.add(DVE, lambda e, dc=dc, r=r: [e.tensor_tensor(
                        v3(tmpB[:, r, npr:n]), v3(tmpB[:, r, npr:n]), bc_seq(gsc2[:, dc, 1:NQ]), ALU.mult)],
                        reads=[("slb", r), "gsc2"], writes=[("slb", r)], name="n2s_b")
                    S.add(DVE, lambda e, dc=dc, r=r: [e.tensor_tensor(
                        v3(h2f[:, dc, npr:n]), v3(tmpB[:, r, npr:n]), bc_seq(modT[:, 3 * 8 + dc, 1:NQ]), ALU.add)],
                        reads=[("slb", r), ] + mod_keys(3), writes=[("h2f", dc)], name="n2s_c")
            for dc in range(NDC):
                S.add(DVE, lambda e, dc=dc: [e.tensor_copy(h2T[:, dc, st.t0:st.t0 + n], h2f[:, dc, 0:n])],
                      reads=[("h2f", dc)], writes=[("h2T", st.idx, dc)], name="h2T_cast")

        def n2_C(st):
            n = st.n
            lbank = 2 + (st.idx % 2)
            S.add(PE, lambda e, lbank=lbank: [e.matmul(ps_all[0:20, lbank, 0:n], wr_sb[:, kc, :], h2f[:, kc, 0:n],
                                                       start=(kc == 0), stop=(kc == NDC - 1)) for kc in range(NDC)],
                  reads=[("h2f", dc) for dc in range(NDC)] + ["wr_sb"], writes=[("ps", lbank)], name="router_mm")
            S.add(ACT, lambda e, lbank=lbank: [e.activation(out=LT[:, 0:n], in_=ps_all[0:20, lbank, 0:n], func=AF.Copy)],
                  reads=[("ps", lbank)], writes=["LT"], name="LT_ev")
            ntile = (n + 127) // 128
            for ti in range(ntile):
                rows = min(128, n - ti * 128)
                gt = st.t0 // 128 + ti
                tb = 4 + (gt % 4)
                S.add(PE, lambda e, ti=ti, rows=rows, tb=tb: [e.matmul(ps_all[0:rows, tb, 0:20], LT[:, ti * 128:ti * 128 + rows],
                                                                        identF[0:20, 0:20], start=True, stop=True)],
                      reads=["LT", "identF"], writes=[("ps", tb)], name="L_tr")
                S.add(DVE, lambda e, rows=rows, gt=gt, tb=tb: [e.tensor_tensor(L[0:rows, gt, :], ps_all[0:rows, tb, 0:20], vecs[0:rows, V_BR:V_BR + 20], ALU.add)],
                      reads=[("ps", tb), "L", "vecs"], writes=[("L", gt)], name="L_ev")

        def route(st):
            g0 = st.t0 // 128
            nt = (st.n + 127) // 128
            g1 = g0 + nt
            Lk = [("L", g) for g in range(g0, g1)] + ["L"]
            G = L[:, g0:g1, 0:4]
            E4 = L[:, g0:g1, 4:20].rearrange("p t (g j) -> p t g j", g=4)
            K = lambda nm: ("rt", nm, st.idx)

            ops = []

            def dv(fn, reads, writes, name):
                ops.append(lambda: S.add(DVE, lambda e: [fn(e)], reads=reads, writes=writes, name=name))

            def b3(ap2, k):
                return ap2.unsqueeze(2).to_broadcast([128, nt, k])

            gm, gk, gs, gu, pgs = gmax[:, g0:g1], gmask[:, g0:g1, :], gsh[:, g0:g1, :], gsum[:, g0:g1], pg[:, g0:g1]
            pn, em, mm1, mm2 = pen[:, g0:g1, :], Em[:, g0:g1, :], m1[:, g0:g1, :], m2[:, g0:g1, :]
            vv1, vv2, dds, ww1, ww2 = v1[:, g0:g1], v2[:, g0:g1], dd[:, g0:g1], w1s[:, g0:g1], w2s[:, g0:g1]
            dv(lambda e: e.tensor_reduce(gm, G, AX.X, ALU.max), Lk, [K("gmax")], "r_gmax")
            dv(lambda e: e.tensor_tensor(gk, G, b3(gm, 4), ALU.is_equal), Lk + [K("gmax")], [K("gmask")], "r_gmask")
            dv(lambda e: e.tensor_tensor(gs, G, b3(gm, 4), ALU.subtract), Lk + [K("gmax")], [K("gsh")], "r_gsh")
            ops.append(lambda: S.add(ACT, lambda e: [e.activation(out=gs, in_=gs, func=AF.Exp)], reads=[K("gsh")], writes=[K("gsh")], name="r_gexp"))
            dv(lambda e: e.tensor_reduce(gu, gs, AX.X, ALU.add), [K("gsh")], [K("gsum")], "r_gsum")
            dv(lambda e: e.reciprocal(pgs, gu), [K("gsum")], [K("pg")], "r_pg")
            dv(lambda e: e.tensor_scalar(pn, gk, BIG, -BIG, ALU.mult, ALU.add), [K("gmask")], [K("pen")], "r_pen")
            dv(lambda e: e.tensor_tensor(em.rearrange("p t (g j) -> p t g j", g=4), E4,
                                         pn.unsqueeze(3).to_broadcast([128, nt, 4, 4]), ALU.add), Lk + [K("pen")], [K("Em")], "r_Em")
            dv(lambda e: e.tensor_reduce(vv1, em, AX.X, ALU.max), [K("Em")], [K("v1")], "r_v1")
            dv(lambda e: e.tensor_tensor(mm1, em, b3(vv1, 16), ALU.is_equal), [K("Em"), K("v1")], [K("m1")], "r_m1")
            dv(lambda e: e.scalar_tensor_tensor(em, mm1, -BIG, em, ALU.mult, ALU.add), [K("m1"), K("Em")], [K("Em")], "r_Em2")
            dv(lambda e: e.tensor_reduce(vv2, em, AX.X, ALU.max), [K("Em")], [K("v2")], "r_v2")
            dv(lambda e: e.tensor_tensor(mm2, em, b3(vv2, 16), ALU.is_equal), [K("Em"), K("v2")], [K("m2")], "r_m2")
            dv(lambda e: e.tensor_tensor(dds, vv2, vv1, ALU.subtract), [K("v1"), K("v2")], [K("dd")], "r_dd")
            ops.append(lambda: S.add(ACT, lambda e: [e.activation(out=ww2, in_=dds, func=AF.Sigmoid)], reads=[K("dd")], writes=[K("w2s")], name="r_w2s"))
            dv(lambda e: e.tensor_scalar(ww1, ww2, -1.0, 1.0, ALU.mult, ALU.add), [K("w2s")], [K("w1s")], "r_w1s")
            dv(lambda e: e.tensor_tensor(ww1, ww1, pgs, ALU.mult), [K("w1s"), K("pg")], [K("w1s")], "r_w1p")
            dv(lambda e: e.tensor_tensor(ww2, ww2, pgs, ALU.mult), [K("w2s"), K("pg")], [K("w2s")], "r_w2p")
            dv(lambda e: e.tensor_tensor(mm1, mm1, b3(ww1, 16), ALU.mult), [K("m1"), K("w1s")], [K("m1")], "r_c1")
            dv(lambda e: e.tensor_tensor(mm2, mm2, b3(ww2, 16), ALU.mult), [K("m2"), K("w2s")], [K("m2")], "r_c2")
            dv(lambda e: e.tensor_tensor(mm1, mm1, mm2, ALU.add), [K("m1"), K("m2")], [K("m1"), K("comb")], "r_comb")
            return ops

        def comb_T(st):
            n = st.n
            ntile = (n + 127) // 128
            bank = 4 + (cb_state["n"] % 2)
            cb_state["n"] += 1

            def mm(e):
                out = []
                for ti in range(ntile):
                    rows = min(128, st.n - ti * 128)
                    gt = st.t0 // 128 + ti
                    out.append(e.matmul(ps_all[0:16, bank, ti * 128:ti * 128 + rows], comb[0:rows, gt, :], identF[0:rows, 0:rows], start=True, stop=True))
                return out
            S.add(PE, mm, reads=[("rt", "comb", st.idx), "identF"], writes=[("ps", bank)], name="combT_mm")
            S.add(ACT, lambda e: [e.activation(out=combT[:, st.t0:st.t0 + st.n], in_=ps_all[0:16, bank, 0:st.n], func=AF.Copy)],
                  reads=[("ps", bank)], writes=[("combT", st.idx)], name="combT_ev")

        cb_state = {"n": 0}

        hb_state = {"n": 0}
        yb_state = {"n": 0}

        def moe_H(p, st, par):
            n = st.n
            slot = p % NWS
            ab = abuf[par]
            for el in range(2):
                e_ = 2 * p + el
                cbk = 4 + (cb_state["n"] % 2)
                cb_state["n"] += 1
                S.add(PE, lambda e, e_=e_, cbk=cbk: [e.matmul(PS(cbk, n), sel[:, e_, :], combT[:, st.t0:st.t0 + n], start=True, stop=True)],
                      reads=["sel", ("combT", st.idx)], writes=[("ps", cbk)], name="comb_bc_mm")
                for f in range(2):
                    hb = (hb_state["n"] % 2) * 2
                    hb_state["n"] += 1
                    S.add(PE, lambda e, el=el, f=f, hb=hb: [e.matmul(PS(hb, n), wA[slot][:, el, kc, f * 128:(f + 1) * 128], h2T[:, kc, st.t0:st.t0 + n],
                                                                     start=(kc == 0), stop=(kc == NDC - 1)) for kc in range(NDC)],
                          reads=[("wexp", slot, el)] + [("h2T", st.idx, dc) for dc in range(NDC)], writes=[("ps", hb)], name="h1_mm")
                    S.add(PE, lambda e, el=el, f=f, hb=hb: [e.matmul(PS(hb + 1, n), wB[slot][:, el, kc, f * 128:(f + 1) * 128], h2T[:, kc, st.t0:st.t0 + n],
                                                                     start=(kc == 0), stop=(kc == NDC - 1)) for kc in range(NDC)],
                          reads=[("wexp", slot, el)] + [("h2T", st.idx, dc) for dc in range(NDC)], writes=[("ps", hb + 1)], name="h3_mm")
                    r = f
                    S.add(ACT, lambda e, hb=hb, r=r: [e.activation(out=slb[:, r, 0:n], in_=PS(hb, n), func=AF.Silu)],
                          reads=[("ps", hb)], writes=[("slb", r)], name="silu_ev")
                    S.add(DVE, lambda e, hb=hb, r=r: [e.tensor_tensor(tlb[:, r, 0:n], PS(hb + 1, n), slb[:, r, 0:n], ALU.mult)],
                          reads=[("ps", hb + 1), ("slb", r)], writes=[("tlb", r)], name="a_ev1")
                    S.add(DVE, lambda e, cbk=cbk, r=r, el=el, f=f: [e.tensor_tensor(ab[:, el * 2 + f, 0:n], PS(cbk, n), tlb[:, r, 0:n], ALU.mult)],
                          reads=[("ps", cbk), ("tlb", r)], writes=[("sqB", par * 4 + el * 2 + f)], name="a_ev2")

        def moe_Y(p, st, par, filler=()):
            n = st.n
            slot = p % NWS
            ab = abuf[par]
            filler = list(filler)
            for m in range(NDC):
                for _ in range(3):
                    if filler:
                        filler.pop(0)()
                yb = 6 + (yb_state["n"] % 2)
                yb_state["n"] += 1

                def mm(e, m=m, yb=yb):
                    out = []
                    i = 0
                    for el in range(2):
                        for f in range(2):
                            out.append(e.matmul(PS(yb, n), wC[slot][:, el, f, m * 128:(m + 1) * 128], ab[:, el * 2 + f, 0:n], start=(i == 0), stop=(i == 3)))
                            i += 1
                    return out
                S.add(PE, mm, reads=[("wexp", slot, 0), ("wexp", slot, 1)] + [("sqB", par * 4 + i) for i in range(4)],
                      writes=[("ps", yb)], name="y_mm")
                npr = st.npr
                xs = xT[:, m, st.t0:st.t0 + npr]
                S.add(DVE, lambda e, m=m, yb=yb, xs=xs, npr=npr: [e.scalar_tensor_tensor(xs, PS(yb, n)[:, 0:npr], mod_scalar(5, m), xs, ALU.mult, ALU.add)],
                      reads=[("ps", yb), ("x", st.idx, m), ] + mod_keys(5), writes=[("x", st.idx, m)], name="x3_ev")
                if npr < n:
                    xs2 = xT[:, m, st.t0 + npr:st.t0 + n]
                    S.add(DVE, lambda e, m=m, yb=yb, npr=npr: [e.tensor_tensor(v3(gtmp[:, npr:n]), v3(PS(yb, n)[:, npr:n]), bc_seq(modT[:, 5 * 8 + m, 1:NQ]), ALU.mult)],
                          reads=[("ps", yb), ] + mod_keys(5), writes=["gtmp"], name="x3s_a")
                    S.add(DVE, lambda e, xs2=xs2, npr=npr: [e.tensor_tensor(xs2, xs2, gtmp[:, npr:n], ALU.add)],
                          reads=["gtmp", ("x", st.idx, m)], writes=[("x", st.idx, m)], name="x3s_b")

            while filler:
                filler.pop(0)()

        sqC = h2f[:].bitcast(BF16)

        def final_norm(st):
            n = st.n
            sqk = [("h2f", i) for i in range(8)]
            S.add(ACT, lambda e: [e.activation(out=sqC[:, :, 0:n], in_=xT[:, :, st.t0:st.t0 + n], func=AF.Square)],
                  reads=[("x", st.idx, dc) for dc in range(NDC)], writes=sqk, name="n3_sq")
            bank = 4 + (cb_state["n"] % 2)
            cb_state["n"] += 1
            S.add(PE, lambda e: [e.matmul(PS(bank, n), c1024[:], sqC[:, dc, 0:n], start=(dc == 0), stop=(dc == NDC - 1)) for dc in range(NDC)],
                  reads=sqk + ["c1024"], writes=[("ps", bank)], name="n3_ss")
            S.add(ACT, lambda e: [e.activation(out=rstdB[:, 0:n], in_=PS(bank, n), func=AF.Ln, bias=EPS, scale=1.0)],
                  reads=[("ps", bank)], writes=["rstdB"], name="n3_ln")
            S.add(ACT, lambda e: [e.activation(out=rstdB[:, 0:n], in_=rstdB[:, 0:n], func=AF.Exp, scale=-0.5)], reads=["rstdB"], writes=["rstdB"], name="n3_exp")
            for dc in range(NDC):
                xs = xT[:, dc, st.t0:st.t0 + n]
                S.add(DVE, lambda e, xs=xs, dc=dc: [e.scalar_tensor_tensor(xs, xs, vcol(V_FNG + dc), rstdB[:, 0:n], ALU.mult, ALU.mult)],
                      reads=[("x", st.idx, dc), "vecs", "rstdB"], writes=[("x", st.idx, dc)], name="n3_y")
            S.add(SP, lambda e: [e.dma_start(out=yT_dv[:, :, st.t0:st.t0 + n], in_=xT[:, :, st.t0:st.t0 + n])],
                  reads=[("x", st.idx, dc) for dc in range(NDC)], writes=[("out", "y", st.idx)], dma=("out", "y", st.idx), ndma=1, name="st_y")

        NP = NE // 2
        NS = len(SUPERTILES)
        ST_ = SUPERTILES
        last_order = [ST_[4], ST_[1], ST_[2], ST_[3], ST_[0]]
        seqB = [(p, st) for p in range(NP - 1) for st in SUPERTILES] + [(NP - 1, st) for st in last_order]
        n2_A(ST_[0])
        n2_A2(ST_[0])
        n2_A(ST_[1])
        n2_B(ST_[0])
        n2_A2(ST_[1])
        n2_C(ST_[0])
        for op_ in route(ST_[0]):
            op_()
        comb_T(ST_[0])
        n2_B(ST_[1])
        moe_H(0, ST_[0], 0)
        for i, (p, st) in enumerate(seqB):
            if i + 1 < len(seqB):
                p1, st1 = seqB[i + 1]
                if p1 == 0:
                    if st1.idx + 1 < NS:
                        n2_A(ST_[st1.idx + 1])
                    n2_C(st1)
                    moe_Y(p, st, i % 2, filler=route(st1))
                    if st1.idx + 1 < NS:
                        n2_A2(ST_[st1.idx + 1])
                    comb_T(st1)
                    if st1.idx + 1 < NS:
                        n2_B(ST_[st1.idx + 1])
                    moe_H(p1, st1, (i + 1) % 2)
                else:
                    moe_H(p1, st1, (i + 1) % 2)
                    moe_Y(p, st, i % 2)
            else:
                moe_Y(p, st, i % 2)
            if p == NP - 1:
                j = i - (NP - 1) * NS
                if j >= 2:
                    final_norm(last_order[j - 2])
                if j == NS - 1:
                    final_norm(last_order[j - 1])
                    final_norm(st)
            if i % NS == NS - 1 and p + 2 < NP:
                load_pair(p + 2)

        S.add(SP, lambda e: [], reads=[k for k in S.last_w.keys() if isinstance(k, tuple) and k[0] == "out"], writes=["__done__"], name="final_wait")

        S.finalize()
        eng_sems = {eng: es.enter_context(nc.semaphore("sem_" + eng)) for eng in ENGINES}
        dma_sems = {}
        for i, key in enumerate(S.dma_count.keys()):
            dma_sems[key] = es.enter_context(nc.semaphore("dsem%d" % i))
        block = es.enter_context(nc.Block())

        @block.tensor
        def _(e):
            S.emit_engine(PE, e, eng_sems, dma_sems)

        @block.scalar
        def _(e):
            S.emit_engine(ACT, e, eng_sems, dma_sems)

        @block.vector
        def _(e):
            S.emit_engine(DVE, e, eng_sems, dma_sems)

        @block.gpsimd
        def _(e):
            S.emit_engine(POOL, e, eng_sems, dma_sems)

        @block.sync
        def _(e):
            S.emit_engine(SP, e, eng_sems, dma_sems)

    return nc


_NC_CACHE = {}


def _prep_inputs(inputs):
    f32 = np.float32
    g = lambda k: np.asarray(inputs[k], dtype=f32)
    x_prompt, x_sample = g("x_prompt"), g("x_sample")
    c_prompt, c_sample = g("c_prompt"), g("c_sample")
    sca, scb = g("state_conv_a")[0], g("state_conv_b")[0]

    vecs = np.zeros((128, NV), f32)
    vecs[:, V_BADA:V_BADA + 48] = g("b_ada")[0].reshape(48, 128).T
    vecs[:, V_N1G:V_N1G + 8] = g("norm1_g")[0].reshape(8, 128).T
    vecs[:, V_N2G:V_N2G + 8] = g("norm2_g")[0].reshape(8, 128).T
    vecs[:, V_FNG:V_FNG + 8] = g("final_norm_g").reshape(8, 128).T
    caw = g("conv_a_w")[0]
    vecs[:, V_CAW:V_CAW + 12] = caw.reshape(3, 4, 128).transpose(2, 1, 0).reshape(128, 12)
    cbw = g("conv_b_w")[0]
    vecs[:, V_CBW:V_CBW + 124] = cbw.reshape(31, 4, 128).transpose(2, 1, 0).reshape(128, 124)
    vecs[:, V_CBB:V_CBB + 4] = g("conv_b_bias")[0].reshape(4, 128).T
    vecs[:, V_LNG:V_LNG + 4] = g("ln_b_g")[0].reshape(4, 128).T
    vecs[:, V_LNB:V_LNB + 4] = g("ln_b_b")[0].reshape(4, 128).T
    br = np.concatenate([g("b_group")[0], g("b_expert")[0]])
    vecs[:, V_BR:V_BR + 20] = br[None, :]
    wr = np.concatenate([g("w_group")[0], g("w_expert")[0]], axis=1)
    wr_l = np.ascontiguousarray(wr.reshape(8, 128, 20).transpose(1, 0, 2).reshape(128, 160))
    shared = {
        "vecs": vecs, "ident": np.eye(128, dtype=f32),
        "w_ada": g("w_ada")[0], "w_in": g("w_in")[0], "w_out_a": g("w_out_a")[0], "w_out_b": g("w_out_b")[0],
        "w_o": g("w_o")[0], "wr": wr_l, "w1": g("w1")[0], "w3": g("w3")[0], "w2": g("w2")[0],
    }
    in_maps = []
    for i in range(NCORES):
        xs = x_sample[16 * i:16 * i + 16].reshape(64, D)
        xt = np.concatenate([x_prompt[i], xs], axis=0)
        xT = np.ascontiguousarray(xt.reshape(NT, 8, 128).transpose(2, 1, 0).reshape(128, 8 * NT))
        c17 = np.concatenate([c_prompt[i:i + 1], c_sample[16 * i:16 * i + 16]], axis=0)
        cT = np.ascontiguousarray(c17.reshape(NQ, 8, 128).transpose(2, 1, 0).reshape(128, 8 * NQ))
        sa = sca[16 * i:16 * i + 16]
        sa_l = np.ascontiguousarray(sa.reshape(16, 2, 4, 128).transpose(3, 2, 0, 1).reshape(128, 4 * 16 * 2))
        sbb = scb[16 * i:16 * i + 16]
        sb_l = np.ascontiguousarray(sbb.reshape(16, 30, 4, 128).transpose(3, 2, 0, 1).reshape(128, 4 * 16 * 30))
        m = dict(shared)
        m.update({"xT": xT, "cT": cT, "sa": sa_l, "sb": sb_l})
        in_maps.append(m)
    return in_maps


def kernel(**inputs):
    if "nc" not in _NC_CACHE:
        _NC_CACHE["nc"] = build_program()
    nc = _NC_CACHE["nc"]
    in_maps = _prep_inputs(inputs)
    res = run_bass_kernel_spmd(nc, in_maps, core_ids=list(range(NCORES)))
    f32 = np.float32
    y_prompt = np.empty((8, SEQ, D), f32)
    y_sample = np.empty((128, TS, D), f32)
    nap = np.empty((1, 8, 2, 512), f32)
    nbp = np.empty((1, 8, 30, 512), f32)
    nas = np.empty((1, 128, 2, 512), f32)
    nbs = np.empty((1, 128, 30, 512), f32)
    for i in range(NCORES):
        r = res.results[i]
        yT = np.asarray(r["yT"], dtype=f32).reshape(128, 8, NT)
        yt = yT.transpose(2, 1, 0).reshape(NT, D)
        y_prompt[i] = yt[:SEQ]
        y_sample[16 * i:16 * i + 16] = yt[SEQ:].reshape(16, TS, D)
        nap[0, i] = np.asarray(r["nap"], f32).reshape(128, 4, 2).transpose(2, 1, 0).reshape(2, 512)
        nbp[0, i] = np.asarray(r["nbp"], f32).reshape(128, 4, 30).transpose(2, 1, 0).reshape(30, 512)
        nas[0, 16 * i:16 * i + 16] = np.asarray(r["nas"], f32).reshape(128, 4, 16, 2).transpose(2, 3, 1, 0).reshape(16, 2, 512)
        nbs[0, 16 * i:16 * i + 16] = np.asarray(r["nbs"], f32).reshape(128, 4, 16, 30).transpose(2, 3, 1, 0).reshape(16, 30, 512)
    return (y_prompt, y_sample, nap, nbp, nas, nbs)
```
